# Optimizing a Trainium2 kernel written in Bass

```python
import jax, jax.numpy as jnp
from jax import lax
import numpy as np

D_MODEL = 1024
BATCH = 4
SEQ = 4096
DEPTH = 1

D_MIX = D_MODEL
MLA_HEADS = 4
QK_NOPE_DIM = 128
QK_ROPE_DIM = 64
QK_HEAD_DIM = QK_NOPE_DIM + QK_ROPE_DIM
V_HEAD_DIM = 128
Q_LORA_RANK = 256
KV_LORA_RANK = 128
ROPE_THETA = 10000.0
ATTN_Q_BLOCK = 128
MLA_WIDTH = MLA_HEADS * V_HEAD_DIM
SG_HEADS = 4
SG_HEAD_DIM = 128
SG_WIDTH = SG_HEADS * SG_HEAD_DIM
SG_CHUNK = 128
IN_COLS = Q_LORA_RANK + KV_LORA_RANK + QK_ROPE_DIM + 2 * SG_WIDTH
PEER_HEADS = 8
PEER_N_KEYS = 128
PEER_N_EXPERTS = PEER_N_KEYS * PEER_N_KEYS
PEER_TOPK = 16
PEER_QUERY_DIM = 256
PEER_HALF_DIM = PEER_QUERY_DIM // 2
PEER_TOKEN_BLOCK = 128
N_MOD = 6
EPS = 1e-6

kernel_name = "hymba_mla_gmlp_peer_adaln_encoder"


def rmsnorm(x, g):
    xf = x.astype(jnp.float32)
    y = xf * lax.rsqrt(jnp.mean(xf * xf, axis=-1, keepdims=True) + EPS)
    return (y * g.astype(jnp.float32)).astype(x.dtype)


def modulate(h, shift, scale):
    return h * (1.0 + scale[:, None, :]) + shift[:, None, :]


def rope(x, cos, sin):
    half = x.shape[-1] // 2
    x1, x2 = x[..., :half], x[..., half:]
    return jnp.concatenate([x1 * cos - x2 * sin, x2 * cos + x1 * sin], axis=-1)


def rope_tables(positions, dtype):
    inv_freq = 1.0 / (ROPE_THETA ** (jnp.arange(0, QK_ROPE_DIM, 2, dtype=jnp.float32) / QK_ROPE_DIM))
    ang = positions.astype(jnp.float32)[..., None] * inv_freq
    return jnp.cos(ang).astype(dtype), jnp.sin(ang).astype(dtype)


def mla_group(c_q, c_kv, k_rope_raw, cos, sin, g_q_a, w_uq, g_kv_a, w_ukv):
    B, S, _ = c_q.shape
    q = (rmsnorm(c_q, g_q_a) @ w_uq).reshape(B, S, MLA_HEADS, QK_HEAD_DIM)
    q_rope = rope(q[..., QK_NOPE_DIM:], cos[:, :, None, :], sin[:, :, None, :])
    q = jnp.concatenate([q[..., :QK_NOPE_DIM], q_rope], axis=-1)
    kv = (rmsnorm(c_kv, g_kv_a) @ w_ukv).reshape(B, S, MLA_HEADS, QK_NOPE_DIM + V_HEAD_DIM)
    k_nope, v = kv[..., :QK_NOPE_DIM], kv[..., QK_NOPE_DIM:]
    k_rope = rope(k_rope_raw, cos, sin)
    k = jnp.concatenate(
        [k_nope, jnp.broadcast_to(k_rope[:, :, None, :], (B, S, MLA_HEADS, QK_ROPE_DIM))], axis=-1)
    scale = QK_HEAD_DIM ** -0.5
    nb = S // ATTN_Q_BLOCK
    qb = q.reshape(B, nb, ATTN_Q_BLOCK, MLA_HEADS, QK_HEAD_DIM).transpose(1, 0, 2, 3, 4)

    def attend(q_blk):
        s = jnp.einsum('bqhd,bkhd->bhqk', q_blk, k).astype(jnp.float32) * scale
        p = jax.nn.softmax(s, axis=-1).astype(v.dtype)
        return jnp.einsum('bhqk,bkhd->bqhd', p, v)

    o = lax.map(attend, qb)
    return o.transpose(1, 0, 2, 3, 4).reshape(B, S, MLA_WIDTH)


def sgu_group(u, v, g_sg, w_sg, b_sg):
    B, S, _ = u.shape
    u = jax.nn.gelu(u, approximate=False)
    v = rmsnorm(jax.nn.gelu(v, approximate=False), g_sg)
    nc = S // SG_CHUNK
    v = v.reshape(B, nc, SG_CHUNK, SG_HEADS, SG_HEAD_DIM)
    mix = jnp.einsum('hpq,bnqhd->bnphd', w_sg, v) + b_sg.T[None, None, :, :, None]
    out = u.reshape(B, nc, SG_CHUNK, SG_HEADS, SG_HEAD_DIM) * mix
    return out.reshape(B, S, SG_WIDTH)


def peer(h, w_q, keys, U, V):
    B, S, D = h.shape
    T = B * S
    ht = h.reshape(T, D)
    q = (ht @ w_q).reshape(T, PEER_HEADS, 2, PEER_HALF_DIM)
    s1 = jnp.einsum('thd,hkd->thk', q[:, :, 0], keys[:, 0]).astype(jnp.float32)
    s2 = jnp.einsum('thd,hkd->thk', q[:, :, 1], keys[:, 1]).astype(jnp.float32)
    v1, i1 = lax.top_k(s1, PEER_TOPK)
    v2, i2 = lax.top_k(s2, PEER_TOPK)
    cand = (v1[..., :, None] + v2[..., None, :]).reshape(T, PEER_HEADS, PEER_TOPK * PEER_TOPK)
    sc, ci = lax.top_k(cand, PEER_TOPK)
    idx = (jnp.take_along_axis(i1, ci // PEER_TOPK, axis=-1) * PEER_N_KEYS
           + jnp.take_along_axis(i2, ci % PEER_TOPK, axis=-1))
    gates = jax.nn.softmax(sc, axis=-1).astype(h.dtype)

    nblk = T // PEER_TOKEN_BLOCK
    hb = ht.reshape(nblk, PEER_TOKEN_BLOCK, D)
    ib = idx.reshape(nblk, PEER_TOKEN_BLOCK, PEER_HEADS, PEER_TOPK)
    gb = gates.reshape(nblk, PEER_TOKEN_BLOCK, PEER_HEADS, PEER_TOPK)

    def experts(args):
        xb, ids, g = args
        u_sel = U[ids]
        a = jnp.einsum('chkd,cd->chk', u_sel, xb)
        w = jax.nn.gelu(a, approximate=False) * g
        return jnp.einsum('chk,chkd->cd', w, V[ids])

    out = lax.map(experts, (hb, ib, gb))
    return out.reshape(B, S, D)


def setup_inputs(seed: int = 0) -> dict:
    key = jax.random.key(seed)
    ks = jax.random.split(key, 26)
    nrm = lambda k, shape, s: jax.random.normal(k, shape, jnp.float32) * s
    gain = lambda k, shape: 1.0 + 0.02 * jax.random.normal(k, shape, jnp.float32)
    L = DEPTH
    positions = (jnp.arange(SEQ, dtype=jnp.int32)[None, :]
                 + jax.random.randint(ks[2], (BATCH, 1), 0, 1024, dtype=jnp.int32))
    return {
        "x": nrm(ks[0], (BATCH, SEQ, D_MODEL), 1.0),
        "c": nrm(ks[1], (BATCH, D_MODEL), 1.0),
        "positions": positions,
        "w_ada": nrm(ks[3], (L, D_MODEL, N_MOD * D_MODEL), 0.5 * D_MODEL ** -0.5),
        "b_ada": nrm(ks[4], (L, N_MOD * D_MODEL), 0.02),
        "g_norm1": gain(ks[5], (L, D_MODEL)),
        "w_in": nrm(ks[6], (L, D_MODEL, IN_COLS), D_MODEL ** -0.5),
        "g_q_a": gain(ks[7], (L, Q_LORA_RANK)),
        "w_uq": nrm(ks[8], (L, Q_LORA_RANK, MLA_HEADS * QK_HEAD_DIM), Q_LORA_RANK ** -0.5),
        "g_kv_a": gain(ks[9], (L, KV_LORA_RANK)),
        "w_ukv": nrm(ks[10], (L, KV_LORA_RANK, MLA_HEADS * (QK_NOPE_DIM + V_HEAD_DIM)), KV_LORA_RANK ** -0.5),
        "g_sg": gain(ks[11], (L, SG_WIDTH)),
        "w_sg": nrm(ks[12], (L, SG_HEADS, SG_CHUNK, SG_CHUNK), SG_CHUNK ** -0.5),
        "b_sg": 1.0 + nrm(ks[13], (L, SG_HEADS, SG_CHUNK), 0.1),
        "g_attn_out": gain(ks[14], (L, MLA_WIDTH)),
        "g_sg_out": gain(ks[15], (L, SG_WIDTH)),
        "w_o": nrm(ks[16], (L, D_MIX, D_MODEL), D_MIX ** -0.5),
        "g_norm2": gain(ks[17], (L, D_MODEL)),
        "w_peer_q": nrm(ks[18], (L, D_MODEL, PEER_HEADS * PEER_QUERY_DIM), D_MODEL ** -0.5),
        "peer_keys": nrm(ks[19], (L, PEER_HEADS, 2, PEER_N_KEYS, PEER_HALF_DIM), PEER_HALF_DIM ** -0.5),
        "peer_u": nrm(ks[20], (L, PEER_N_EXPERTS, D_MODEL), D_MODEL ** -0.5),
        "peer_v": nrm(ks[21], (L, PEER_N_EXPERTS, D_MODEL), PEER_HEADS ** -0.5),
        "g_final": gain(ks[22], (D_MODEL,)),
    }


def reference(x, c, positions, w_ada, b_ada, g_norm1, w_in, g_q_a, w_uq, g_kv_a, w_ukv,
              g_sg, w_sg, b_sg, g_attn_out, g_sg_out, w_o, g_norm2, w_peer_q, peer_keys,
              peer_u, peer_v, g_final):
    cos, sin = rope_tables(positions, x.dtype)
    c_act = jax.nn.silu(c)
    o1 = Q_LORA_RANK
    o2 = o1 + KV_LORA_RANK
    o3 = o2 + QK_ROPE_DIM
    o4 = o3 + SG_WIDTH
    for l in range(DEPTH):
        mod = c_act @ w_ada[l] + b_ada[l]
        shift1, scale1, gate1, shift2, scale2, gate2 = jnp.split(mod, N_MOD, axis=-1)
        h = modulate(rmsnorm(x, g_norm1[l]), shift1, scale1)
        z = h @ w_in[l]
        y_attn = mla_group(z[..., :o1], z[..., o1:o2], z[..., o2:o3], cos, sin,
                           g_q_a[l], w_uq[l], g_kv_a[l], w_ukv[l])
        y_sg = sgu_group(z[..., o3:o4], z[..., o4:], g_sg[l], w_sg[l], b_sg[l])
        y = jnp.concatenate([rmsnorm(y_attn, g_attn_out[l]), rmsnorm(y_sg, g_sg_out[l])], axis=-1)
        x = x + gate1[:, None, :] * (y @ w_o[l])
        h2 = modulate(rmsnorm(x, g_norm2[l]), shift2, scale2)
        x = x + gate2[:, None, :] * peer(h2, w_peer_q[l], peer_keys[l], peer_u[l], peer_v[l])
    return rmsnorm(x, g_final)
```

```python
import numpy as np
import concourse.bass as bass
import concourse.mybir as mybir
from contextlib import ExitStack

F32 = mybir.dt.float32
BF16 = mybir.dt.bfloat16
I32 = mybir.dt.int32
U32 = mybir.dt.uint32
U16 = mybir.dt.uint16
AF = mybir.ActivationFunctionType
ALU = mybir.AluOpType
AX = mybir.AxisListType

ENGS = ["pe", "act", "dve", "pool", "sp"]


class Buf:
    def __init__(self, name, t):
        self.name = name
        self.t = t
        self.last_write = None
        self.reads = {}

    def __getitem__(self, k):
        return self.t[k]


class Prog:
    def __init__(self, nc, es):
        self.nc = nc
        self.es = es
        self.ops = {e: [] for e in ENGS}
        self.cnt = {e: 0 for e in ENGS}
        self.sem = {e: es.enter_context(nc.semaphore("sem_" + e)) for e in ENGS}
        self.waited = {e: {} for e in ENGS}
        self.dma_sems = {}
        self.dma_cnt = {}
        self.semobj = {}
        for e in ENGS:
            self.semobj[("eng", e)] = self.sem[e]
        self.nbuf = 0
        import os
        self.limit = int(os.environ.get('OPLIMIT', '0')) or None
        self.total = 0
        self.log = []

    def sb(self, name, shape, dtype):
        t = self.es.enter_context(self.nc.sbuf_tensor("s_" + name, list(shape), dtype))
        return Buf(name, t)

    def ps(self, name, shape, dtype=F32):
        t = self.es.enter_context(self.nc.psum_tensor("p_" + name, list(shape), dtype))
        return Buf(name, t)

    def dma_sem(self, key):
        if key not in self.dma_sems:
            s = self.es.enter_context(self.nc.semaphore("dsem_%s" % key))
            self.dma_sems[key] = s
            self.dma_cnt[key] = 0
            self.semobj[("dma", key)] = s
        return self.dma_sems[key]

    def _deps(self, eng, reads, writes, is_dma):
        raw = set()
        war = set()
        for b in reads:
            if b.last_write is not None:
                raw.add(b.last_write)
        for b in writes:
            if b.last_write is not None:
                raw.add(b.last_write)
            war.update((k[0], k[1], v) for k, v in b.reads.items())
        deps = set(raw)
        for d in war:
            if d[0] == "eng" and d[1] == eng and not is_dma and eng == "pe":
                continue
            deps.add(d)
        if eng == "pe" and not is_dma:
            deps = {d for d in deps if not (d[0] == "eng" and d[1] == "pe")}
        return deps

    def _emit_waits(self, eng, deps):
        waits = []
        w = self.waited[eng]
        best = {}
        for (kind, key, val) in deps:
            k = (kind, key)
            if w.get(k, 0) >= val:
                continue
            if best.get(k, 0) < val:
                best[k] = val
        for k, val in best.items():
            w[k] = val
            waits.append((self.semobj[k], val))
        return waits

    def op(self, eng, fn, reads=(), writes=(), same_eng_war=False):
        self.total += 1
        import sys as _s
        fr = _s._getframe(1)
        ln = []
        while fr is not None and len(ln) < 3:
            ln.append(fr.f_lineno)
            fr = fr.f_back
        self.log.append((self.total, eng, ln))
        if self.limit is not None and self.total > self.limit:
            return ("eng", eng, self.cnt[eng])
        reads = [b for b in reads if b is not None]
        writes = [b for b in writes if b is not None]
        deps = self._deps(eng, reads, writes, False)
        waits = self._emit_waits(eng, deps)
        self.cnt[eng] += 1
        val = self.cnt[eng]
        self.ops[eng].append((waits, fn, (self.sem[eng], 1)))
        tag = ("eng", eng, val)
        for b in writes:
            b.last_write = tag
            b.reads = {}
        for b in reads:
            if b not in writes:
                b.reads[(tag[0], tag[1])] = max(b.reads.get((tag[0], tag[1]), 0), tag[2])
        return tag

    def dma(self, eng, fn, key, reads=(), writes=()):
        self.total += 1
        if self.limit is not None and self.total > self.limit:
            self.dma_sem(key)
            return ("dma", key, self.dma_cnt[key])
        reads = [b for b in reads if b is not None]
        writes = [b for b in writes if b is not None]
        sem = self.dma_sem(key)
        deps = self._deps(eng, reads, writes, True)
        waits = self._emit_waits(eng, deps)
        self.dma_cnt[key] += 16
        val = self.dma_cnt[key]
        self.ops[eng].append((waits, fn, (sem, 16)))
        tag = ("dma", key, val)
        for b in writes:
            b.last_write = tag
            b.reads = {}
        for b in reads:
            if b not in writes:
                b.reads[(tag[0], tag[1])] = max(b.reads.get((tag[0], tag[1]), 0), tag[2])
        return tag

    def final_wait(self, eng, tags):
        waits = self._emit_waits(eng, tags)
        self.ops[eng].append((waits, None, None))

    def emit(self):
        nc = self.nc
        with nc.Block() as block:
            def run(engh, name):
                for (waits, fn, inc) in self.ops[name]:
                    for (s, v) in waits:
                        engh.wait_ge(s, v)
                    if fn is not None:
                        ins = fn(engh)
                        ins.then_inc(inc[0], inc[1])

            @block.tensor
            def _(e):
                run(e, "pe")

            @block.scalar
            def _(e):
                run(e, "act")

            @block.vector
            def _(e):
                run(e, "dve")

            @block.gpsimd
            def _(e):
                run(e, "pool")

            @block.sync
            def _(e):
                run(e, "sp")


def _prog_barrier(self):
    tags = [("eng", e, self.cnt[e]) for e in ENGS if self.cnt[e] > 0]
    tags += [("dma", key, v) for key, v in self.dma_cnt.items() if v > 0]
    for e in ENGS:
        waits = self._emit_waits(e, [t for t in tags if not (t[0] == "eng" and t[1] == e)])
        if waits:
            self.ops[e].append((waits, None, None))


Prog.barrier = _prog_barrier


from concourse.bass_utils import run_bass_kernel_spmd

EPS = 1e-6
NT_OWN = 16
NT_SEQ = 32
DBG = False


class K:
    def __init__(self, P):
        self.P = P

    def act(self, out, in_, func, R, W, **kw):
        return self.P.op("act", lambda e: e.activation(out=out, in_=in_, func=func, **kw), R, W)

    def ts(self, out, in0, s1, s2, op0, op1, R, W, eng="dve"):
        if op1 is None:
            return self.P.op(eng, lambda e: e.tensor_scalar(out=out, in0=in0, scalar1=s1, scalar2=None, op0=op0), R, W)
        return self.P.op(eng, lambda e: e.tensor_scalar(out=out, in0=in0, scalar1=s1, scalar2=s2, op0=op0, op1=op1), R, W)

    def tt(self, out, in0, in1, op, R, W, eng="dve"):
        return self.P.op(eng, lambda e: e.tensor_tensor(out=out, in0=in0, in1=in1, op=op), R, W)

    def stt(self, out, in0, scalar, in1, op0, op1, R, W, accum_out=None):
        if accum_out is None:
            return self.P.op("dve", lambda e: e.scalar_tensor_tensor(out=out, in0=in0, scalar=scalar, in1=in1, op0=op0, op1=op1), R, W)
        return self.P.op("dve", lambda e: e.scalar_tensor_tensor(out=out, in0=in0, scalar=scalar, in1=in1, op0=op0, op1=op1, accum_out=accum_out), R, W)

    def cp(self, out, in_, R, W, eng="dve"):
        return self.P.op(eng, lambda e: e.tensor_copy(out=out, in_=in_), R, W)

    def recip(self, out, in_, R, W):
        return self.P.op("dve", lambda e: e.reciprocal(out=out, in_=in_), R, W)

    def mm(self, out, lhsT, rhs, start, stop, R, W):
        return self.P.op("pe", lambda e: e.matmul(out=out, lhsT=lhsT, rhs=rhs, start=start, stop=stop, skip_group_check=True), R, W)

    def tr(self, out, in_, ident, R, W):
        return self.P.op("pe", lambda e: e.transpose(out=out, in_=in_, identity=ident), R, W)

    def dma(self, q, out, in_, key, R, W):
        return self.P.dma(q, lambda e: e.dma_start(out=out, in_=in_), key, R, W)

    def gather(self, out, table, idx_ap, key, R, W):
        return self.P.dma("pool", lambda e: e.indirect_dma_start(out=out, out_offset=None, in_=table, in_offset=bass.IndirectOffsetOnAxis(ap=idx_ap, axis=0)), key, R, W)

    def memset(self, ap, val, W, eng="dve"):
        return self.P.op(eng, lambda e: e.memset(ap, val), [], W)

    def rstd(self, st, n):
        self.act(st[:, 1:2], st[:, 0:1], AF.Sqrt, [st], [st], scale=1.0 / n, bias=self.epsb[:, 0:1])
        self.recip(st[:, 1:2], st[:, 1:2], [st], [st])


class _Done(Exception):
    pass


def build_program(stop=None, dumps=()):
    nc = bass.Bass("TRN2", target_bir_lowering=False)
    small_peer = stop is not None

    def din(name, shape, dt=F32):
        return nc.dram_tensor(name, list(shape), dt, kind="ExternalInput").ap()

    xs = din("xs", [4096, 1024])
    pos_d = din("pos", [128, 32], I32)
    invf_d = din("invf", [128, 32])
    c_d = din("c_l", [128, 8])
    wada_d = din("w_ada", [1024, 6144])
    bada_d = din("b_ada", [128, 48])
    g1_d = din("g1", [128, 8])
    g2_d = din("g2", [128, 8])
    gq_d = din("gq", [128, 2])
    gkv_d = din("gkv", [128, 1])
    gout_d = din("gout", [128, 8])
    gsg_d = din("gsg", [1, 512])
    gfin_d = din("gfin", [1, 1024])
    bsg_d = din("bsgT", [128, 4])
    win_d = din("w_in", [1024, 1472])
    wuq_d = din("w_uq", [256, 768])
    wukv_d = din("w_ukv", [128, 1024])
    wsg_d = din("w_sgT", [128, 4, 128])
    wo_d = din("w_o", [1024, 1024])
    wpq_d = din("w_pq", [1024, 2048])
    keys_d = din("keysT", [128, 16, 128])
    put_d = din("peer_uT", [1024, 128 if small_peer else 16384])
    pv_d = din("peer_v", [128 if small_peer else 16384, 1024])
    out_d = nc.dram_tensor("out", [2048, 1024], F32, kind="ExternalOutput").ap()
    scr_d = nc.dram_tensor("scr", [56, 128], F32, kind="Internal").ap()
    scr_flat = scr_d.rearrange("a b -> (a b)")

    try:
      with ExitStack() as es0:
        P = Prog(nc, es0)
        k = K(P)
        scrB = Buf("scr", scr_d)
        reg = {}

        def finish(phase):
            if stop != phase:
                return
            tags = []
            for i, nm in enumerate(dumps):
                b = reg[nm]
                ap = b[:]
                od = nc.dram_tensor("dbg_" + nm, list(ap.shape), ap.dtype, kind="ExternalOutput").ap()
                tags.append(P.dma("sp", (lambda o, a: lambda e: e.dma_start(out=o, in_=a))(od, ap), "dbg%d" % i, [b], []))
            P.final_wait("sp", tags)
            print("total ops at finish", P.total, {e: P.cnt[e] for e in ENGS})
            import os
            if os.environ.get('SHOWLOG'):
                a, b = [int(v) for v in os.environ['SHOWLOG'].split(',')]
                for it in P.log:
                    if a <= it[0] <= b:
                        print(it)
            P.emit()
            raise _Done()

        identb = P.sb("identb", [128, 128], BF16)
        identf = P.sb("identf", [128, 128], F32)
        iotf = P.sb("iotf", [128, 128], F32)
        iot16 = P.sb("iot16", [128, 16], F32)
        fm = P.sb("fm", [128, 56], F32)
        g1s = P.sb("g1s", [128, 8], F32)
        smallw = P.sb("smallw", [128, 32], F32)
        bsgT = P.sb("bsgT_sb", [128, 4], F32)
        sts = [P.sb("st%d" % i, [128, 2], F32) for i in range(6)]
        epsb = P.sb("epsb", [128, 1], F32)
        k.memset(epsb[:], EPS, [epsb])
        k.epsb = epsb
        Y = P.sb("Y", [128, 16, 1024], BF16)

        P.op("pool", lambda e: e.iota(iotf[:], pattern=[[1, 128]], base=0, channel_multiplier=-1, allow_small_or_imprecise_dtypes=True), [], [iotf])
        P.op("pool", lambda e: e.iota(iot16[:], pattern=[[1, 16]], base=0, channel_multiplier=0, allow_small_or_imprecise_dtypes=True), [], [iot16])
        P.op("dve", lambda e: e.tensor_single_scalar(out=identf[:], in_=iotf[:], scalar=0.0, op=ALU.is_equal), [iotf], [identf])
        k.cp(identb[:], identf[:], [identf], [identb])
        k.dma("sp", smallw[:, 0:8], g1_d, "smallw", [], [smallw])
        k.dma("sp", smallw[:, 8:16], g2_d, "smallw", [], [smallw])
        k.dma("sp", smallw[:, 16:18], gq_d, "smallw", [], [smallw])
        k.dma("sp", smallw[:, 18:19], gkv_d, "smallw", [], [smallw])
        k.dma("sp", smallw[:, 19:27], gout_d, "smallw", [], [smallw])
        k.dma("sp", bsgT[:], bsg_d, "bsg", [], [bsgT])

        with ExitStack() as es:
            P.es = es
            cact = P.sb("cact", [128, 8, 2], F32)
            cl = P.sb("cl", [128, 8], F32)
            bada = P.sb("bada", [128, 48], F32)
            wa = [P.sb("wa%d" % i, [128, 8, 512], F32) for i in range(2)]
            modp = P.ps("modp", [128, 48, 2], F32)
            fmT_p = P.ps("fmT_p", [56, 128], F32)
            fmT = P.sb("fmT", [56, 128], F32)
            k.dma("sp", cl[:], c_d, "cl", [], [cl])
            k.dma("sp", bada[:], bada_d, "bada", [], [bada])
            k.act(cact[:, :, 0], cl[:], AF.Silu, [cl], [cact])
            k.act(cact[:, :, 1], cl[:], AF.Silu, [cl], [cact])
            wada_v = wada_d.rearrange("(k p) c -> p k c", p=128)
            for j in range(12):
                w = wa[j % 2]
                k.dma("sp", w[:], wada_v[:, :, j * 512:(j + 1) * 512], "wa%d" % (j % 2), [], [w])
                for jj in range(4):
                    col = j * 4 + jj
                    for kc in range(8):
                        k.mm(modp[:, col, :], w[:, kc, jj * 128:(jj + 1) * 128], cact[:, kc, :], kc == 0, kc == 7, [w, cact], [modp])
            k.tt(fm[:, 0:48], modp[:, :, 0], bada[:], ALU.add, [modp, bada], [fm])
            k.stt(g1s[:], fm[:, 8:16], 1.0, smallw[:, 0:8], ALU.add, ALU.mult, [fm, smallw], [g1s])
            k.stt(fm[:, 48:56], fm[:, 32:40], 1.0, smallw[:, 8:16], ALU.add, ALU.mult, [fm, smallw], [fm])
            k.tr(fmT_p[:], fm[:, 0:56], identf[:], [fm, identf], [fmT_p])
            k.cp(fmT[:], fmT_p[:], [fmT_p], [fmT])
            k.dma("sp", scr_d, fmT[:], "scrw", [fmT], [scrB])
            P.barrier()
            reg.update(fm=fm, g1s=g1s)
            finish("0")
        P.es = es0

        def bc_row(name, lo, n):
            t = P.sb(name, [128, n], F32)
            k.dma("sp", t[:], scr_flat[lo:lo + n].partition_broadcast(128), name, [scrB], [t])
            return t

        with ExitStack() as es12:
            P.es = es12
            KTn = P.sb("KTn", [128, 4, 4096], BF16)
            Vsb = P.sb("Vsb", [128, 32, 4, 129], BF16)
            KTr = P.sb("KTr", [128, 4096], BF16)
            cqT = P.sb("cqT", [128, 2, 2048], BF16)
            cosT = P.sb("cosT", [128, 32, 32], F32)
            sinT = P.sb("sinT", [128, 32, 32], F32)
            ssqa = P.sb("ssqa", [128, 16, 2], F32)
            k.memset(Vsb[:, :, :, 128:129], 1.0, [Vsb])
            k.memset(ssqa[:], 0.0, [ssqa])

            with ExitStack() as es:
                P.es = es
                posi = P.sb("posi", [128, 32], I32)
                posf = P.sb("posf", [128, 32], F32)
                invf = P.sb("invf_sb", [128, 32], F32)
                ang = P.sb("ang", [128, 32, 32], F32)
                angi = P.sb("angi", [128, 32, 32], I32)
                angr = P.sb("angr", [128, 32, 32], F32)
                k.dma("sp", posi[:], pos_d, "posi", [], [posi])
                k.dma("sp", invf[:], invf_d, "invf", [], [invf])
                k.cp(posf[:], posi[:], [posi], [posf])
                k.tt(ang[:], posf[:].unsqueeze(2).to_broadcast([128, 32, 32]), invf[:].unsqueeze(1).to_broadcast([128, 32, 32]), ALU.mult, [posf, invf], [ang])
                for (dst, off) in ((sinT, 0.0), (cosT, 0.25)):
                    if off != 0.0:
                        k.ts(ang[:], ang[:], off, None, ALU.add, None, [ang], [ang])
                    k.cp(angi[:], ang[:], [ang], [angi])
                    k.cp(angr[:], angi[:], [angi], [angr])
                    k.tt(angr[:], ang[:], angr[:], ALU.subtract, [ang, angr], [angr])
                    k.act(dst[:], angr[:], AF.Sin, [angr], [dst], scale=6.28318)
                P.barrier()
                reg.update(cosT=cosT, sinT=sinT)
                finish("rope")
            P.es = es12

            with ExitStack() as es:
                P.es = es
                Win = P.sb("Win", [128, 8, 1472], BF16)
                Wukv = P.sb("Wukv", [128, 1024], BF16)
                WsgT = P.sb("WsgT", [128, 4, 128], BF16)
                gsgbc = P.sb("gsgbc", [128, 512], F32)
                xt = [P.sb("xt%d" % i, [128, 1024], F32) for i in range(2)]
                xn = P.sb("xn", [128, 1024], BF16)
                hT = P.sb("hT", [128, 8, 128], BF16)
                z = P.sb("z", [128, 1472], F32)
                junk = P.sb("junk", [128, 512], BF16)
                ckvn = P.sb("ckvn", [128, 128], BF16)
                ckvT = P.sb("ckvT", [128, 128], BF16)
                kr = P.sb("kr", [128, 128], BF16)
                rt = [P.sb("rt%d" % i, [128, 32], F32) for i in range(4)]
                cqn = P.sb("cqn", [128, 256], BF16)
                gu = P.sb("gu", [128, 512], F32)
                gv = P.sb("gv", [128, 512], F32)
                vn = P.sb("vn", [128, 512], BF16)
                ysg = P.sb("ysg", [128, 512], F32)
                tp = P.ps("tp", [128, 8, 128], BF16)
                zp = P.ps("zp", [128, 1536], F32)
                tpx = P.ps("tpx", [128, 4, 128], BF16)
                kp = P.ps("kp", [128, 4, 128], F32)
                vp = P.ps("vp", [128, 512], F32)
                mp = P.ps("mp", [128, 4, 128], F32)
                st_x, st_kv, st_q, st_v, st_sg = sts[0], sts[1], sts[2], sts[3], sts[4]

                k.memset(kr[:], 0.0, [kr])
                k.dma("pool", Win[:], win_d.rearrange("(k p) c -> p k c", p=128), "Win", [], [Win])
                k.dma("pool", Wukv[:], wukv_d, "Wukv", [], [Wukv])
                k.dma("pool", WsgT[:], wsg_d, "WsgT", [], [WsgT])
                k.dma("sp", gsgbc[:], gsg_d.partition_broadcast(128), "gsgbc", [], [gsgbc])
                k.ts(Wukv[:], Wukv[:], smallw[:, 18:19], None, ALU.mult, None, [Wukv, smallw], [Wukv])

                for n in range(NT_SEQ):
                    own = n < NT_OWN
                    x_ = xt[n % 2]
                    k.dma("sp", x_[:], xs[n * 128:(n + 1) * 128, :], "xt%d" % (n % 2), [], [x_])
                    k.act(xn[:], x_[:], AF.Square, [x_], [xn, st_x], accum_out=st_x[:, 0:1])
                    k.rstd(st_x, 1024)
                    k.act(xn[:], x_[:], AF.Identity, [x_, st_x], [xn], scale=st_x[:, 1:2])
                    for kc in range(8):
                        k.tr(tp[:, kc, :], xn[:, kc * 128:(kc + 1) * 128], identb[:], [xn, identb], [tp])
                    for kc in range(8):
                        k.ts(hT[:, kc, :], tp[:, kc, :], g1s[:, kc:kc + 1], fm[:, kc:kc + 1], ALU.mult, ALU.add, [tp, g1s, fm], [hT])
                    chunks = ((0, 512), (512, 1024), (1024, 1472)) if own else ((256, 448),)
                    for (lo, hi) in chunks:
                        for kc in range(8):
                            k.mm(zp[:, lo:hi], hT[:, kc, :], Win[:, kc, lo:hi], kc == 0, kc == 7, [hT, Win], [zp])
                    for (lo, hi) in chunks:
                        k.act(z[:, lo:hi], zp[:, lo:hi], AF.Identity, [zp], [z])
                    k.act(junk[:, 0:128], z[:, 256:384], AF.Square, [z], [junk, st_kv], accum_out=st_kv[:, 0:1])
                    k.rstd(st_kv, 128)
                    k.act(ckvn[:], z[:, 256:384], AF.Identity, [z, st_kv], [ckvn], scale=st_kv[:, 1:2])
                    cs, sn = cosT[:, n, :], sinT[:, n, :]
                    x1, x2 = z[:, 384:416], z[:, 416:448]
                    k.tt(rt[0][:], x1, cs, ALU.mult, [z, cosT], [rt[0]])
                    k.tt(rt[1][:], x2, sn, ALU.mult, [z, sinT], [rt[1]])
                    k.tt(kr[:, 0:32], rt[0][:], rt[1][:], ALU.subtract, [rt[0], rt[1]], [kr])
                    k.tt(rt[2][:], x2, cs, ALU.mult, [z, cosT], [rt[2]])
                    k.tt(rt[3][:], x1, sn, ALU.mult, [z, sinT], [rt[3]])
                    k.tt(kr[:, 32:64], rt[2][:], rt[3][:], ALU.add, [rt[2], rt[3]], [kr])
                    k.tr(tpx[:, 0, :], ckvn[:], identb[:], [ckvn, identb], [tpx])
                    k.tr(tpx[:, 1, :], kr[:], identb[:], [kr, identb], [tpx])
                    k.cp(ckvT[:], tpx[:, 0, :], [tpx], [ckvT])
                    k.cp(KTr[:, n * 128:(n + 1) * 128], tpx[:, 1, :], [tpx], [KTr])
                    for h in range(4):
                        k.mm(kp[:, h, :], Wukv[:, h * 128:(h + 1) * 128], ckvT[:], True, True, [Wukv, ckvT], [kp])
                    k.mm(vp[:], ckvT[:], Wukv[:, 512:1024], True, True, [Wukv, ckvT], [vp])
                    k.act(KTn[:, :, n * 128:(n + 1) * 128], kp[:], AF.Identity, [kp], [KTn])
                    k.cp(Vsb[:, n, :, 0:128], vp[:].rearrange("p (h d) -> p h d", h=4), [vp], [Vsb])
                    if not own:
                        continue
                    k.act(junk[:, 0:256], z[:, 0:256], AF.Square, [z], [junk, st_q], accum_out=st_q[:, 0:1])
                    k.rstd(st_q, 256)
                    k.act(cqn[:], z[:, 0:256], AF.Identity, [z, st_q], [cqn], scale=st_q[:, 1:2])
                    for j in range(2):
                        k.tr(tpx[:, 2 + j, :], cqn[:, j * 128:(j + 1) * 128], identb[:], [cqn, identb], [tpx])
                    k.cp(cqT[:, :, n * 128:(n + 1) * 128], tpx[:, 2:4, :], [tpx], [cqT])
                    k.act(gu[:], z[:, 448:960], AF.Gelu, [z], [gu])
                    k.act(gv[:], z[:, 960:1472], AF.Gelu, [z], [gv])
                    k.act(junk[:], gv[:], AF.Square, [gv], [junk, st_v], accum_out=st_v[:, 0:1])
                    k.rstd(st_v, 512)
                    k.stt(vn[:], gv[:], st_v[:, 1:2], gsgbc[:], ALU.mult, ALU.mult, [gv, st_v, gsgbc], [vn])
                    for h in range(4):
                        k.mm(mp[:, h, :], WsgT[:, h, :], vn[:, h * 128:(h + 1) * 128], True, True, [WsgT, vn], [mp])
                    for h in range(4):
                        k.stt(ysg[:, h * 128:(h + 1) * 128], mp[:, h, :], bsgT[:, h:h + 1], gu[:, h * 128:(h + 1) * 128], ALU.add, ALU.mult, [mp, bsgT, gu], [ysg])
                    k.act(junk[:], ysg[:], AF.Square, [ysg], [junk, st_sg], accum_out=st_sg[:, 0:1])
                    k.rstd(st_sg, 512)
                    k.act(Y[:, n, 512:1024], ysg[:], AF.Identity, [ysg, st_sg], [Y], scale=st_sg[:, 1:2])
                P.barrier()
                reg.update(KTn=KTn, Vsb=Vsb, KTr=KTr, cqT=cqT, Y=Y)
                finish("1a")
            P.es = es12

            with ExitStack() as es:
                P.es = es
                Wuq = P.sb("Wuq", [128, 2, 768], BF16)
                QTn2 = [P.sb("QTn%d" % i, [128, 4, 512], BF16) for i in range(2)]
                QTr2 = [P.sb("QTr%d" % i, [128, 4, 512], BF16) for i in range(2)]
                qbr = P.sb("qbr", [128, 4, 128], BF16)
                qsb = P.sb("qsb", [128, 4, 192], F32)
                qb = P.sb("qb", [128, 4, 192], BF16)
                qt_ = [P.sb("qt%d" % i, [128, 4, 32], F32) for i in range(4)]
                PT = [P.sb("PT%d" % i, [128, 512], BF16) for i in range(4)]
                ya = P.sb("ya", [128, 128], F32)
                osb = P.sb("osb", [128, 4, 129], F32)
                rc = P.sb("rc", [128, 2], F32)
                jk2 = P.sb("jk2", [128, 128], BF16)
                qp = P.ps("qp", [128, 1024], F32)
                tq = P.ps("tq", [128, 8, 128], BF16)
                Sp = [P.ps("Sp%d" % i, [128, 512], F32) for i in range(3)]
                Op = P.ps("Op", [128, 4, 256], F32)
                st_a = sts[0]
                k.memset(qbr[:], 0.0, [qbr])
                k.dma("pool", Wuq[:], wuq_d.rearrange("(j p) c -> p j c", p=128), "Wuq", [], [Wuq])
                for j in range(2):
                    k.ts(Wuq[:, j, :], Wuq[:, j, :], smallw[:, 16 + j:17 + j], None, ALU.mult, None, [Wuq, smallw], [Wuq])
                sc = 192.0 ** -0.5
                si = 0
                def Qcomp(T):
                    QTn, QTr = QTn2[T % 2], QTr2[T % 2]
                    for jj in range(4):
                        n = T * 4 + jj
                        for (lo, hi) in ((0, 512), (512, 768)):
                            for j in range(2):
                                k.mm(qp[:, lo:hi], cqT[:, j, n * 128:(n + 1) * 128], Wuq[:, j, lo:hi], j == 0, j == 1, [cqT, Wuq], [qp])
                        k.act(qsb[:].rearrange("p h d -> p (h d)"), qp[:, 0:768], AF.Identity, [qp], [qsb], scale=sc)
                        cs = cosT[:, n, :].unsqueeze(1).to_broadcast([128, 4, 32])
                        sn = sinT[:, n, :].unsqueeze(1).to_broadcast([128, 4, 32])
                        x1, x2 = qsb[:, :, 128:160], qsb[:, :, 160:192]
                        k.tt(qt_[0][:], x1, cs, ALU.mult, [qsb, cosT], [qt_[0]])
                        k.tt(qt_[1][:], x2, sn, ALU.mult, [qsb, sinT], [qt_[1]])
                        k.tt(qbr[:, :, 0:32], qt_[0][:], qt_[1][:], ALU.subtract, [qt_[0], qt_[1]], [qbr])
                        k.tt(qt_[2][:], x2, cs, ALU.mult, [qsb, cosT], [qt_[2]])
                        k.tt(qt_[3][:], x1, sn, ALU.mult, [qsb, sinT], [qt_[3]])
                        k.tt(qbr[:, :, 32:64], qt_[2][:], qt_[3][:], ALU.add, [qt_[2], qt_[3]], [qbr])
                        k.act(qb[:, :, 0:128], qsb[:, :, 0:128], AF.Identity, [qsb], [qb])
                        for h in range(4):
                            k.tr(tq[:, h, :], qb[:, h, 0:128], identb[:], [qb, identb], [tq])
                            k.tr(tq[:, 4 + h, :], qbr[:, h, :], identb[:], [qbr, identb], [tq])
                        k.cp(QTn[:, :, jj * 128:(jj + 1) * 128], tq[:, 0:4, :], [tq], [QTn])
                        k.cp(QTr[:, :, jj * 128:(jj + 1) * 128], tq[:, 4:8, :], [tq], [QTr])

                Qcomp(0)
                for T in range(4):
                    QTn, QTr = QTn2[T % 2], QTr2[T % 2]
                    items = [(h, tk) for h in range(4) for tk in range(32)]

                    def emitS(q):
                        h, tk = items[q]
                        S = Sp[q % 3]
                        pt = PT[q % 4]
                        k.mm(S[:], KTn[:, h, tk * 128:(tk + 1) * 128], QTn[:, h, :], True, False, [KTn, QTn], [S])
                        k.mm(S[:], KTr[:, tk * 128:(tk + 1) * 128], QTr[:, h, :], False, True, [KTr, QTr], [S])
                        k.act(pt[:], S[:], AF.Exp, [S], [pt])

                    def emitPV(q):
                        h, tk = items[q]
                        pt = PT[q % 4]
                        for jj in range(4):
                            k.mm(Op[:, jj, 0:129], pt[:, jj * 128:(jj + 1) * 128], Vsb[:, tk, h, :], (tk == 0 and jj in (0, 2)), tk == 31, [pt, Vsb], [Op])
                        if tk == 31:
                            k.cp(osb[:], Op[:, :, 0:129], [Op], [osb])
                            for jj in range(4):
                                n = T * 4 + jj
                                k.recip(rc[:, 0:1], osb[:, jj, 128:129], [osb], [rc])
                                k.ts(ya[:], osb[:, jj, 0:128], rc[:, 0:1], None, ALU.mult, None, [osb, rc], [ya])
                                k.act(jk2[:], ya[:], AF.Square, [ya], [jk2, rc], accum_out=rc[:, 1:2])
                                k.tt(ssqa[:, n, 0:1], ssqa[:, n, 0:1], rc[:, 1:2], ALU.add, [ssqa, rc], [ssqa])
                                k.cp(Y[:, n, h * 128:(h + 1) * 128], ya[:], [ya], [Y])

                    emitS(0)
                    emitS(1)
                    for q in range(len(items)):
                        if q + 2 < len(items):
                            emitS(q + 2)
                        emitPV(q)
                        if q == 40 and T + 1 < 4:
                            Qcomp(T + 1)
                for n in range(NT_OWN):
                    k.act(st_a[:, 1:2], ssqa[:, n, 0:1], AF.Sqrt, [ssqa, epsb], [st_a], scale=1.0 / 512, bias=epsb[:, 0:1])
                    k.recip(st_a[:, 1:2], st_a[:, 1:2], [st_a], [st_a])
                    k.act(Y[:, n, 0:512], Y[:, n, 0:512], AF.Identity, [Y, st_a], [Y], scale=st_a[:, 1:2])
                P.barrier()
                finish("2")
            P.es = es12
        P.es = es0

        x1 = P.sb("x1", [128, 16, 1024], F32)
        x1b = [Buf("x1_%d" % n, x1.t[:, n, :]) for n in range(NT_OWN)]
        rstd2 = P.sb("rstd2", [128, 16], F32)
        with ExitStack() as es:
            P.es = es
            Wo = P.sb("Wo", [128, 8, 1024], BF16)
            g1bc = bc_row("g1bc", 16 * 128, 1024)
            xt = [P.sb("xt3_%d" % i, [128, 1024], F32) for i in range(2)]
            YT2 = [P.sb("YT%d" % i, [128, 8, 128], BF16) for i in range(2)]
            tmp = P.sb("tmp3", [128, 1024], F32)
            tp2 = [P.ps("tp3_%d" % i, [128, 8, 128], BF16) for i in range(2)]
            op2 = [P.ps("op3_%d" % i, [128, 1024], F32) for i in range(2)]
            k.dma("pool", Wo[:], wo_d.rearrange("(k p) c -> p k c", p=128), "Wo", [], [Wo])
            for kc in range(8):
                k.ts(Wo[:, kc, :], Wo[:, kc, :], smallw[:, 19 + kc:20 + kc], None, ALU.mult, None, [Wo, smallw], [Wo])

            def p3A(n):
                x_ = xt[n % 2]
                k.dma("sp", x_[:], xs[n * 128:(n + 1) * 128, :], "xt3_%d" % (n % 2), [], [x_])
                for kc in range(8):
                    k.tr(tp2[n % 2][:, kc, :], Y[:, n, kc * 128:(kc + 1) * 128], identb[:], [Y, identb], [tp2[n % 2]])
                k.cp(YT2[n % 2][:], tp2[n % 2][:], [tp2[n % 2]], [YT2[n % 2]])

            def p3B(n):
                x_ = xt[n % 2]
                YT, op_ = YT2[n % 2], op2[n % 2]
                for c in range(2):
                    for kc in range(8):
                        k.mm(op_[:, c * 512:(c + 1) * 512], YT[:, kc, :], Wo[:, kc, c * 512:(c + 1) * 512], kc == 0, kc == 7, [YT, Wo], [op_])
                k.tt(tmp[:], op_[:], g1bc[:], ALU.mult, [op_, g1bc], [tmp])
                k.tt(x1b[n][:], tmp[:], x_[:], ALU.add, [tmp, x_], [x1b[n]])

            p3A(0)
            for n in range(NT_OWN):
                if n + 1 < NT_OWN:
                    p3A(n + 1)
                p3B(n)
            P.barrier()
            reg.update(x1=x1)
            finish("3")
        P.es = es0

        with ExitStack() as es4:
            P.es = es4
            H2T = P.sb("H2T", [128, 8, 2048], BF16)
            es4ab = ExitStack()
            P.es = es4ab
            tabT = P.sb("tabT", [128, 16, 3, 128], BF16)
            P.es = es4
            with ExitStack() as es:
                P.es = es
                Wpq = Buf("Wpq", Y.t[:].rearrange("p a (b c) -> p (a b) c", b=2).rearrange("p (k j) c -> p k (j c)", k=8))
                keysT = P.sb("keysT", [128, 16, 128], F32)
                xn2 = P.sb("xn2", [128, 1024], BF16)
                qTs2 = [P.sb("qTs%d" % i, [128, 16, 128], F32) for i in range(1)] * 2
                ssb2 = [P.sb("ssb%d" % i, [128, 16, 128], F32) for i in range(2)]
                qTs = qTs2[0]
                v16 = P.sb("v16", [128, 8, 2, 16], F32)
                ix = P.sb("ix", [128, 8, 2, 16], U32)
                ixf = P.sb("ixf", [128, 8, 2, 16], F32)
                cand = P.sb("cand", [128, 8, 16, 16], F32)
                scr8 = P.sb("scr8", [128, 8, 256], F32)
                cv = P.sb("cvv", [128, 8, 16], F32)
                pos = P.sb("pos4", [128, 8, 16], U32)
                pa = P.sb("pa", [128, 8, 16], U32)
                pb = P.sb("pb", [128, 8, 16], U32)
                paf = P.sb("paf", [128, 8, 16], F32)
                pbf = P.sb("pbf", [128, 8, 16], F32)
                i1s = P.sb("i1s", [128, 8, 16], F32)
                i2s = P.sb("i2s", [128, 8, 16], F32)
                ex = P.sb("ex", [128, 8, 16], F32)
                zz = P.sb("zz", [128, 8], F32)
                tabm = P.sb("tabm", [128, 3, 128], BF16)
                tp = P.ps("tp4", [128, 8, 128], BF16)
                qp = P.ps("qp4", [128, 16, 128], F32)
                tbp = P.ps("tbp", [128, 3, 128], BF16)
                st2 = sts[0]
                candw = scr8
                v16c = [Buf("v16c%d" % c, v16.t[:, c // 2, c % 2, :]) for c in range(16)]
                ixc = [Buf("ixc%d" % c, ix.t[:, c // 2, c % 2, :]) for c in range(16)]
                swc2 = [[Buf("swc%d_%d" % (i, c), ssb2[i].t[:, c, :]) for c in range(16)] for i in range(2)]
                cvh = [Buf("cvh%d" % h, cv.t[:, h, :]) for h in range(8)]
                posh = [Buf("posh%d" % h, pos.t[:, h, :]) for h in range(8)]
                cwh = [Buf("cwh%d" % h, scr8.t[:, h, :]) for h in range(8)]
                eq4 = Buf("eq4", scr8.t[:].rearrange("p h (a b) -> p h a b", a=16))
                eq4 = scr8
                eqv = scr8.t[:].rearrange("p h (a b) -> p h a b", a=16)
                k.dma("pool", Wpq[:], wpq_d.rearrange("(k p) c -> p k c", p=128), "Wpq", [], [Wpq])
                k.dma("sp", keysT[:], keys_d, "keysT", [], [keysT])
                def front4a(n):
                    xb = x1b[n]
                    qTs, ssb, swc = qTs2[n % 2], ssb2[n % 2], swc2[n % 2]
                    sw = ssb
                    k.act(xn2[:], xb[:], AF.Square, [xb], [xn2, st2], accum_out=st2[:, 0:1])
                    k.rstd(st2, 1024)
                    k.act(xn2[:], xb[:], AF.Identity, [xb, st2], [xn2], scale=st2[:, 1:2])
                    for kc in range(8):
                        k.tr(tp[:, kc, :], xn2[:, kc * 128:(kc + 1) * 128], identb[:], [xn2, identb], [tp])
                    for kc in range(8):
                        k.act(H2T[:, kc, n * 128:(n + 1) * 128], tp[:, kc, :], AF.Identity, [tp, fm], [H2T], scale=fm[:, 48 + kc:49 + kc], bias=fm[:, 24 + kc:25 + kc])
                    for c in range(16):
                        for kc in range(8):
                            k.mm(qp[:, c, :], Wpq[:, kc, c * 128:(c + 1) * 128], H2T[:, kc, n * 128:(n + 1) * 128], kc == 0, kc == 7, [Wpq, H2T], [qp])
                    k.act(qTs[:], qp[:], AF.Identity, [qp], [qTs])
                    for c in range(16):
                        k.mm(qp[:, c, :], qTs[:, c, :], keysT[:, c, :], True, True, [qTs, keysT], [qp])
                    k.act(ssb[:], qp[:], AF.Identity, [qp], [ssb] + swc)

                def back4a(n):
                    qTs, ssb, swc = qTs2[n % 2], ssb2[n % 2], swc2[n % 2]
                    sw = ssb
                    hs = [(c // 2, c % 2) for c in range(16)]
                    for c, (h, s_) in enumerate(hs):
                        P.op("dve", (lambda o, i: lambda e: e.max(out=o, in_=i))(v16[:, h, s_, 0:8], ssb[:, c, :]), [ssb, swc[c]], [v16c[c]])
                    for c, (h, s_) in enumerate(hs):
                        P.op("dve", (lambda o, m, i: lambda e: e.max_index(out=o, in_max=m, in_values=i))(ix[:, h, s_, 0:8], v16[:, h, s_, 0:8], ssb[:, c, :]), [ssb, swc[c], v16c[c]], [ixc[c]])
                    for c, (h, s_) in enumerate(hs):
                        P.op("dve", (lambda o, m, i: lambda e: e.match_replace(out=o, in_to_replace=m, in_values=i, imm_value=-1e30))(sw[:, c, :], v16[:, h, s_, 0:8], ssb[:, c, :]), [ssb, swc[c], v16c[c]], [swc[c]])
                    for c, (h, s_) in enumerate(hs):
                        P.op("dve", (lambda o, i: lambda e: e.max(out=o, in_=i))(v16[:, h, s_, 8:16], sw[:, c, :]), [swc[c]], [v16c[c]])
                    for c, (h, s_) in enumerate(hs):
                        P.op("dve", (lambda o, m, i: lambda e: e.max_index(out=o, in_max=m, in_values=i))(ix[:, h, s_, 8:16], v16[:, h, s_, 8:16], sw[:, c, :]), [swc[c], v16c[c]], [ixc[c]])
                    k.cp(ixf[:], ix[:], ixc, [ixf])
                    k.tt(cand[:], v16[:, :, 0, :].unsqueeze(3).to_broadcast([128, 8, 16, 16]), v16[:, :, 1, :].unsqueeze(2).to_broadcast([128, 8, 16, 16]), ALU.add, v16c, [cand])
                    cfs = [cand[:, h].rearrange("p a b -> p (a b)") for h in range(8)]
                    for h in range(8):
                        P.op("dve", (lambda o, i: lambda e: e.max(out=o, in_=i))(cv[:, h, 0:8], cfs[h]), [cand], [cvh[h]])
                    for h in range(8):
                        P.op("dve", (lambda o, m, i: lambda e: e.max_index(out=o, in_max=m, in_values=i))(pos[:, h, 0:8], cv[:, h, 0:8], cfs[h]), [cand, cvh[h]], [posh[h]])
                    for h in range(8):
                        P.op("dve", (lambda o, m, i: lambda e: e.match_replace(out=o, in_to_replace=m, in_values=i, imm_value=-1e30))(candw[:, h, :], cv[:, h, 0:8], cfs[h]), [cand, cvh[h]], [cwh[h]])
                    for h in range(8):
                        P.op("dve", (lambda o, i: lambda e: e.max(out=o, in_=i))(cv[:, h, 8:16], candw[:, h, :]), [cwh[h]], [cvh[h]])
                    for h in range(8):
                        P.op("dve", (lambda o, m, i: lambda e: e.max_index(out=o, in_max=m, in_values=i))(pos[:, h, 8:16], cv[:, h, 8:16], candw[:, h, :]), [cwh[h], cvh[h]], [posh[h]])
                    P.op("dve", lambda e: e.tensor_single_scalar(out=pa[:], in_=pos[:], scalar=4, op=ALU.logical_shift_right), posh, [pa])
                    P.op("dve", lambda e: e.tensor_single_scalar(out=pb[:], in_=pos[:], scalar=15, op=ALU.bitwise_and), posh, [pb])
                    k.cp(paf[:], pa[:], [pa], [paf])
                    k.cp(pbf[:], pb[:], [pb], [pbf])
                    iob = iot16[:].unsqueeze(1).unsqueeze(1).to_broadcast([128, 8, 16, 16])
                    for (pf, side, dst) in ((paf, 0, i1s), (pbf, 1, i2s)):
                        k.tt(eqv, pf[:].unsqueeze(3).to_broadcast([128, 8, 16, 16]), iob, ALU.is_equal, [pf, iot16], [scr8] + cwh)
                        k.tt(eqv, eqv, ixf[:, :, side, :].unsqueeze(2).to_broadcast([128, 8, 16, 16]), ALU.mult, [scr8, ixf], [scr8])
                        P.op("dve", (lambda o, i: lambda e: e.tensor_reduce(out=o, in_=i, axis=AX.X, op=ALU.add))(dst[:], eqv), [scr8], [dst])
                    k.cp(tabm[:, 0, :], i1s[:].rearrange("p h k -> p (h k)"), [i1s], [tabm])
                    k.cp(tabm[:, 1, :], i2s[:].rearrange("p h k -> p (h k)"), [i2s], [tabm])
                    k.tt(ex[:], cv[:], cv[:, :, 0:1].to_broadcast([128, 8, 16]), ALU.subtract, cvh, [ex])
                    k.act(ex[:], ex[:], AF.Exp, [ex], [ex])
                    P.op("dve", (lambda o, i: lambda e: e.tensor_reduce(out=o, in_=i, axis=AX.X, op=ALU.add))(zz[:], ex[:]), [ex], [zz])
                    k.recip(zz[:], zz[:], [zz], [zz])
                    k.tt(tabm[:, 2, :].rearrange("p (h k) -> p h k", h=8), ex[:], zz[:].unsqueeze(2).to_broadcast([128, 8, 16]), ALU.mult, [ex, zz], [tabm])
                    for q in range(3):
                        k.tr(tbp[:, q, :], tabm[:, q, :], identb[:], [tabm, identb], [tbp])
                    k.cp(tabT[:, n, :, :], tbp[:], [tbp], [tabT])

                front4a(0)
                for n in range(NT_OWN):
                    if n + 1 < NT_OWN:
                        front4a(n + 1)
                    back4a(n)
                P.barrier()
                reg.update(tabT=tabT, H2T=H2T)
                finish("4a")
            P.es = es4

            Gd = nc.dram_tensor("Gd", [16, 128, 128, 128], BF16, kind="Internal").ap()
            GdB = Buf("Gd", Gd)
            with ExitStack() as es:
                P.es = es
                Gt = Buf("Gt", Y.t[:].rearrange("p a (b c) -> p (a b) c", b=8))
                iotab = P.sb("iotab", [128, 128], BF16)
                TB = 16
                Lr = [P.sb("Lr%d" % i, [128, TB, 128], BF16) for i in range(2)]
                Rr = [P.sb("Rr%d" % i, [128, TB, 128], BF16) for i in range(2)]
                gps = [P.ps("gps%d" % i, [128, 8, 128], F32) for i in range(2)]
                Gt1 = P.sb("Gt1", [128, 128, 128], BF16)
                Gts = [Gt, Gt1]
                P.op("pool", lambda e: e.iota(iotab[:], pattern=[[1, 128]], base=0, channel_multiplier=0, allow_small_or_imprecise_dtypes=True), [], [iotab])
                ri = 0
                for n in range(NT_OWN):
                    Gtn = Gts[n % 2]
                    for t0 in range(0, 128, TB):
                        L, R = Lr[ri % 2], Rr[ri % 2]
                        ri += 1
                        for q in range(TB):
                            t = t0 + q
                            k.ts(L[:, q, :], iotab[:], tabT[:, n, 0, t:t + 1], tabT[:, n, 2, t:t + 1], ALU.is_equal, ALU.mult, [iotab, tabT], [L])
                            k.ts(R[:, q, :], iotab[:], tabT[:, n, 1, t:t + 1], None, ALU.is_equal, None, [iotab, tabT], [R])
                        for q in range(TB):
                            t = t0 + q
                            g = gps[(t // 8) % 2]
                            k.mm(g[:, t % 8, :], R[:, q, :], L[:, q, :], True, True, [R, L], [g])
                            if t % 8 == 7:
                                k.act(Gtn[:, :, t - 7:t + 1], g[:].rearrange("j t i -> j i t"), AF.Identity, [g], [Gtn])
                    k.dma("sp", Gd[n], Gtn[:], "Gdw%d" % (n % 2), [Gtn], [GdB])
                Gt = Gts[(NT_OWN - 1) % 2]
                P.barrier()
                reg.update(Gt=Gt)
                finish("4b")
            P.es = es4
            es4ab.close()

            with ExitStack() as es:
                P.es = es
                g2bc = bc_row("g2bc", 40 * 128, 1024)
                gfbc = P.sb("gfbc", [128, 1024], F32)
                k.dma("sp", gfbc[:], gfin_d.partition_broadcast(128), "gfbc", [], [gfbc])
                UTg = [Buf("UTg%d" % b, Y.t[:, 4 * b:4 * b + 4, :].rearrange("p a (b c) -> p (a b) c", b=2)) for b in range(2)]
                Vg = [Buf("Vg%d" % b, Y.t[:, 8 + 4 * b:12 + 4 * b, :]) for b in range(2)]
                xl = [P.sb("xl%d" % i, [128, 1024], F32) for i in range(2)]
                Gg = [P.sb("Gg%d" % i, [128, 16, 4, 128], BF16) for i in range(2)]
                ot = [P.sb("ot%d" % i, [128, 1024], F32) for i in range(2)]
                NAP = 3
                aps = [P.ps("aps%d" % i, [128, 512], F32) for i in range(NAP)]
                accs = [P.ps("acc%d" % i, [128, 1024], F32) for i in range(2)]
                st3 = sts[1]
                X1d = nc.dram_tensor("X1d", [16, 128, 1024], F32, kind="Internal").ap()
                X1dB = Buf("X1d", X1d)
                for n in range(NT_OWN):
                    k.dma("sp", X1d[n], x1b[n][:], "x1sp", [x1b[n]], [X1dB])
                UT_v = put_d.rearrange("(k p) e -> p k e", p=128)
                V_v = pv_d.rearrange("(i j) d -> j i d", j=128)
                Gd_v = Gd.rearrange("n j i t -> j n i t")
                NG = 32

                def load_group(g):
                    b = g % 2
                    k.dma("pool", UTg[b][:], UT_v[:, :, g * 512:(g + 1) * 512], "UTg%d" % b, [], [UTg[b]])
                    k.dma("pool", Vg[b][:], V_v[:, 4 * g:4 * g + 4, :], "Vg%d" % b, [], [Vg[b]])
                    k.dma("act", Gg[b][:], Gd_v[:, :, 4 * g:4 * g + 4, :], "Gg%d" % b, [GdB], [Gg[b]])

                steps = [(g, c) for g in range(NG) for c in range(8)]
                NGL, NWT = 4, 8
                gl = [P.sb("gl2_%d" % i, [128, 256], BF16) for i in range(NGL)]
                wt = [P.sb("wt2_%d" % i, [128, 256], BF16) for i in range(NWT)]

                def Ablock(si):
                    g, c = steps[si]
                    b = g % 2
                    for i in range(4):
                        slot = si * 4 + i
                        ap = aps[slot % NAP]
                        gg, ww = gl[slot % NGL], wt[slot % NWT]
                        for kc in range(8):
                            k.mm(ap[:, 0:256], UTg[b][:, kc, i * 128:(i + 1) * 128], H2T[:, kc, c * 256:(c + 1) * 256], kc == 0, kc == 7, [UTg[b], H2T], [ap])
                        k.act(gg[:], ap[:, 0:256], AF.Gelu, [ap], [gg])
                        k.tt(ww[:].rearrange("p (s t) -> p s t", s=2), gg[:].rearrange("p (s t) -> p s t", s=2), Gg[b][:, 2 * c:2 * c + 2, i, :], ALU.mult, [gg, Gg[b]], [ww])

                def Oblock(si):
                    g, c = steps[si]
                    b = g % 2
                    for s_ in range(2):
                        A = accs[s_]
                        for i in range(4):
                            ww = wt[(si * 4 + i) % NWT]
                            for dc in range(2):
                                k.mm(A[:, dc * 512:(dc + 1) * 512], ww[:, s_ * 128:(s_ + 1) * 128], Vg[b][:, i, dc * 512:(dc + 1) * 512], i == 0, i == 3, [ww, Vg[b]], [A])
                        xb = x1b[2 * c + s_]
                        if g == 0:
                            k.act(xb[:], A[:], AF.Identity, [A], [xb])
                        else:
                            k.tt(xb[:], A[:], xb[:], ALU.add, [A, xb], [xb])

                load_group(0)
                Ablock(0)
                for si in range(len(steps)):
                    g, c = steps[si]
                    if c == 0 and g + 1 < NG:
                        load_group(g + 1)
                    if si + 1 < len(steps):
                        Ablock(si + 1)
                    Oblock(si)
                outs = []
                st3s = [sts[1], sts[2]]

                def finA(n):
                    xb = x1b[n]
                    xo = xl[n % 2]
                    s3 = st3s[n % 2]
                    k.dma("sp", xo[:], X1d[n], "xl%d" % (n % 2), [X1dB], [xo])
                    k.tt(xb[:], xb[:], g2bc[:], ALU.mult, [xb, g2bc], [xb])
                    k.tt(xb[:], xb[:], xo[:], ALU.add, [xb, xo], [xb])
                    k.act(xo[:], xb[:], AF.Square, [xb], [xo, s3], accum_out=s3[:, 0:1])
                    k.act(s3[:, 1:2], s3[:, 0:1], AF.Sqrt, [s3], [s3], scale=1.0 / 1024, bias=epsb[:, 0:1])

                def finB(n):
                    xb = x1b[n]
                    s3 = st3s[n % 2]
                    k.recip(s3[:, 1:2], s3[:, 1:2], [s3], [s3])
                    o = ot[n % 2]
                    k.stt(o[:], xb[:], s3[:, 1:2], gfbc[:], ALU.mult, ALU.mult, [xb, s3, gfbc], [o])
                    outs.append(k.dma("sp", out_d[n * 128:(n + 1) * 128, :], o[:], "ot%d" % (n % 2), [o], []))

                finA(0)
                for n in range(NT_OWN):
                    if n + 1 < NT_OWN:
                        finA(n + 1)
                    finB(n)
                P.final_wait("sp", outs)
            P.es = es4
        P.es = es0
        P.emit()
    except _Done:
        pass
    return nc


def _prep_inputs(inp):
    f32 = np.float32
    x = np.asarray(inp["x"], f32)
    c = np.asarray(inp["c"], f32)
    positions = np.asarray(inp["positions"], np.int32)

    def fmaj(v, n):
        return np.ascontiguousarray(np.asarray(v, f32).reshape(n, 128).T)

    inv_freq = (1.0 / (10000.0 ** (np.arange(0, 64, 2, dtype=np.float32) / 64.0))).astype(f32)
    invf = np.ascontiguousarray(np.tile((inv_freq / np.float32(2 * np.pi)).astype(f32)[None, :], (128, 1)))
    w_ukv = np.asarray(inp["w_ukv"], f32)[0].reshape(128, 4, 2, 128)
    w_ukv_l = np.ascontiguousarray(np.concatenate([w_ukv[:, :, 0, :].reshape(128, 512), w_ukv[:, :, 1, :].reshape(128, 512)], axis=1))
    shared = {
        "invf": invf,
        "w_ada": np.ascontiguousarray(np.asarray(inp["w_ada"], f32)[0]),
        "b_ada": fmaj(np.asarray(inp["b_ada"])[0], 48),
        "g1": fmaj(np.asarray(inp["g_norm1"])[0], 8),
        "g2": fmaj(np.asarray(inp["g_norm2"])[0], 8),
        "gq": fmaj(np.asarray(inp["g_q_a"])[0], 2),
        "gkv": fmaj(np.asarray(inp["g_kv_a"])[0], 1),
        "gout": fmaj(np.concatenate([np.asarray(inp["g_attn_out"])[0], np.asarray(inp["g_sg_out"])[0]]), 8),
        "gsg": np.ascontiguousarray(np.asarray(inp["g_sg"], f32)[0].reshape(1, 512)),
        "gfin": np.ascontiguousarray(np.asarray(inp["g_final"], f32).reshape(1, 1024)),
        "bsgT": np.ascontiguousarray(np.asarray(inp["b_sg"], f32)[0].T),
        "w_in": np.ascontiguousarray(np.asarray(inp["w_in"], f32)[0]),
        "w_uq": np.ascontiguousarray(np.asarray(inp["w_uq"], f32)[0]),
        "w_ukv": w_ukv_l,
        "w_sgT": np.ascontiguousarray(np.asarray(inp["w_sg"], f32)[0].transpose(2, 0, 1)),
        "w_o": np.ascontiguousarray(np.asarray(inp["w_o"], f32)[0]),
        "w_pq": np.ascontiguousarray(np.asarray(inp["w_peer_q"], f32)[0]),
        "keysT": np.ascontiguousarray(np.asarray(inp["peer_keys"], f32)[0].reshape(16, 128, 128).transpose(2, 0, 1)),
        "peer_uT": np.ascontiguousarray(np.asarray(inp["peer_u"], f32)[0].T),
        "peer_v": np.ascontiguousarray(np.asarray(inp["peer_v"], f32)[0]),
    }
    maps = []
    for core in range(8):
        b, half = core // 2, core % 2
        own = slice(half * 2048, (half + 1) * 2048)
        oth = slice((1 - half) * 2048, (2 - half) * 2048)
        m = dict(shared)
        m["xs"] = np.ascontiguousarray(np.concatenate([x[b, own], x[b, oth]], axis=0))
        p = np.concatenate([positions[b, own], positions[b, oth]]).astype(np.int32)
        m["pos"] = np.ascontiguousarray(p.reshape(32, 128).T)
        m["c_l"] = fmaj(c[b], 8)
        maps.append(m)
    return maps


_NC = None


def kernel(**inputs):
    global _NC
    maps = _prep_inputs(inputs)
    if _NC is None:
        _NC = build_program()
    res = run_bass_kernel_spmd(_NC, maps, core_ids=list(range(8)))
    out = np.empty((4, 4096, 1024), np.float32)
    for core in range(8):
        b, half = core // 2, core % 2
        out[b, half * 2048:(half + 1) * 2048, :] = res.results[core]["out"]
    return out
```

```python
import numpy as np
import concourse.bass as bass
import concourse.mybir as mybir
from contextlib import ExitStack

F32 = mybir.dt.float32
BF16 = mybir.dt.bfloat16
I32 = mybir.dt.int32
U32 = mybir.dt.uint32
U16 = mybir.dt.uint16
AF = mybir.ActivationFunctionType
ALU = mybir.AluOpType
AX = mybir.AxisListType

ENGS = ["pe", "act", "dve", "pool", "sp"]


class Buf:
    def __init__(self, name, t):
        self.name = name
        self.t = t
        self.last_write = None
        self.reads = {}

    def __getitem__(self, k):
        return self.t[k]


class Prog:
    def __init__(self, nc, es):
        self.nc = nc
        self.es = es
        self.ops = {e: [] for e in ENGS}
        self.cnt = {e: 0 for e in ENGS}
        self.sem = {e: es.enter_context(nc.semaphore("sem_" + e)) for e in ENGS}
        self.waited = {e: {} for e in ENGS}
        self.dma_sems = {}
        self.dma_cnt = {}
        self.semobj = {}
        for e in ENGS:
            self.semobj[("eng", e)] = self.sem[e]
        self.nbuf = 0
        import os
        self.limit = int(os.environ.get('OPLIMIT', '0')) or None
        self.total = 0
        self.log = []

    def sb(self, name, shape, dtype):
        t = self.es.enter_context(self.nc.sbuf_tensor("s_" + name, list(shape), dtype))
        return Buf(name, t)

    def ps(self, name, shape, dtype=F32):
        t = self.es.enter_context(self.nc.psum_tensor("p_" + name, list(shape), dtype))
        return Buf(name, t)

    def dma_sem(self, key):
        if key not in self.dma_sems:
            s = self.es.enter_context(self.nc.semaphore("dsem_%s" % key))
            self.dma_sems[key] = s
            self.dma_cnt[key] = 0
            self.semobj[("dma", key)] = s
        return self.dma_sems[key]

    def _deps(self, eng, reads, writes, is_dma):
        raw = set()
        war = set()
        for b in reads:
            if b.last_write is not None:
                raw.add(b.last_write)
        for b in writes:
            if b.last_write is not None:
                raw.add(b.last_write)
            war.update((k[0], k[1], v) for k, v in b.reads.items())
        deps = set(raw)
        for d in war:
            if d[0] == "eng" and d[1] == eng and not is_dma and eng == "pe":
                continue
            deps.add(d)
        if eng == "pe" and not is_dma:
            deps = {d for d in deps if not (d[0] == "eng" and d[1] == "pe")}
        return deps

    def _emit_waits(self, eng, deps):
        waits = []
        w = self.waited[eng]
        best = {}
        for (kind, key, val) in deps:
            k = (kind, key)
            if w.get(k, 0) >= val:
                continue
            if best.get(k, 0) < val:
                best[k] = val
        for k, val in best.items():
            w[k] = val
            waits.append((self.semobj[k], val))
        return waits

    def op(self, eng, fn, reads=(), writes=(), same_eng_war=False):
        self.total += 1
        import sys as _s
        fr = _s._getframe(1)
        ln = []
        while fr is not None and len(ln) < 3:
            ln.append(fr.f_lineno)
            fr = fr.f_back
        self.log.append((self.total, eng, ln))
        if self.limit is not None and self.total > self.limit:
            return ("eng", eng, self.cnt[eng])
        reads = [b for b in reads if b is not None]
        writes = [b for b in writes if b is not None]
        deps = self._deps(eng, reads, writes, False)
        waits = self._emit_waits(eng, deps)
        self.cnt[eng] += 1
        val = self.cnt[eng]
        self.ops[eng].append((waits, fn, (self.sem[eng], 1)))
        tag = ("eng", eng, val)
        for b in writes:
            b.last_write = tag
            b.reads = {}
        for b in reads:
            if b not in writes:
                b.reads[(tag[0], tag[1])] = max(b.reads.get((tag[0], tag[1]), 0), tag[2])
        return tag

    def dma(self, eng, fn, key, reads=(), writes=()):
        self.total += 1
        if self.limit is not None and self.total > self.limit:
            self.dma_sem(key)
            return ("dma", key, self.dma_cnt[key])
        reads = [b for b in reads if b is not None]
        writes = [b for b in writes if b is not None]
        sem = self.dma_sem(key)
        deps = self._deps(eng, reads, writes, True)
        waits = self._emit_waits(eng, deps)
        self.dma_cnt[key] += 16
        val = self.dma_cnt[key]
        self.ops[eng].append((waits, fn, (sem, 16)))
        tag = ("dma", key, val)
        for b in writes:
            b.last_write = tag
            b.reads = {}
        for b in reads:
            if b not in writes:
                b.reads[(tag[0], tag[1])] = max(b.reads.get((tag[0], tag[1]), 0), tag[2])
        return tag

    def final_wait(self, eng, tags):
        waits = self._emit_waits(eng, tags)
        self.ops[eng].append((waits, None, None))

    def emit(self):
        nc = self.nc
        with nc.Block() as block:
            def run(engh, name):
                for (waits, fn, inc) in self.ops[name]:
                    for (s, v) in waits:
                        engh.wait_ge(s, v)
                    if fn is not None:
                        ins = fn(engh)
                        ins.then_inc(inc[0], inc[1])

            @block.tensor
            def _(e):
                run(e, "pe")

            @block.scalar
            def _(e):
                run(e, "act")

            @block.vector
            def _(e):
                run(e, "dve")

            @block.gpsimd
            def _(e):
                run(e, "pool")

            @block.sync
            def _(e):
                run(e, "sp")


def _prog_barrier(self):
    tags = [("eng", e, self.cnt[e]) for e in ENGS if self.cnt[e] > 0]
    tags += [("dma", key, v) for key, v in self.dma_cnt.items() if v > 0]
    for e in ENGS:
        waits = self._emit_waits(e, [t for t in tags if not (t[0] == "eng" and t[1] == e)])
        if waits:
            self.ops[e].append((waits, None, None))


Prog.barrier = _prog_barrier


from concourse.bass_utils import run_bass_kernel_spmd

EPS = 1e-6
NT_OWN = 16
NT_SEQ = 32
DBG = False


class K:
    def __init__(self, P):
        self.P = P

    def act(self, out, in_, func, R, W, **kw):
        return self.P.op("act", lambda e: e.activation(out=out, in_=in_, func=func, **kw), R, W)

    def ts(self, out, in0, s1, s2, op0, op1, R, W, eng="dve"):
        if op1 is None:
            return self.P.op(eng, lambda e: e.tensor_scalar(out=out, in0=in0, scalar1=s1, scalar2=None, op0=op0), R, W)
        return self.P.op(eng, lambda e: e.tensor_scalar(out=out, in0=in0, scalar1=s1, scalar2=s2, op0=op0, op1=op1), R, W)

    def tt(self, out, in0, in1, op, R, W, eng="dve"):
        return self.P.op(eng, lambda e: e.tensor_tensor(out=out, in0=in0, in1=in1, op=op), R, W)

    def stt(self, out, in0, scalar, in1, op0, op1, R, W, accum_out=None):
        if accum_out is None:
            return self.P.op("dve", lambda e: e.scalar_tensor_tensor(out=out, in0=in0, scalar=scalar, in1=in1, op0=op0, op1=op1), R, W)
        return self.P.op("dve", lambda e: e.scalar_tensor_tensor(out=out, in0=in0, scalar=scalar, in1=in1, op0=op0, op1=op1, accum_out=accum_out), R, W)

    def cp(self, out, in_, R, W, eng="dve"):
        return self.P.op(eng, lambda e: e.tensor_copy(out=out, in_=in_), R, W)

    def recip(self, out, in_, R, W):
        return self.P.op("dve", lambda e: e.reciprocal(out=out, in_=in_), R, W)

    def mm(self, out, lhsT, rhs, start, stop, R, W):
        return self.P.op("pe", lambda e: e.matmul(out=out, lhsT=lhsT, rhs=rhs, start=start, stop=stop, skip_group_check=True), R, W)

    def tr(self, out, in_, ident, R, W):
        return self.P.op("pe", lambda e: e.transpose(out=out, in_=in_, identity=ident), R, W)

    def dma(self, q, out, in_, key, R, W):
        return self.P.dma(q, lambda e: e.dma_start(out=out, in_=in_), key, R, W)

    def gather(self, out, table, idx_ap, key, R, W):
        return self.P.dma("pool", lambda e: e.indirect_dma_start(out=out, out_offset=None, in_=table, in_offset=bass.IndirectOffsetOnAxis(ap=idx_ap, axis=0)), key, R, W)

    def memset(self, ap, val, W, eng="dve"):
        return self.P.op(eng, lambda e: e.memset(ap, val), [], W)

    def rstd(self, st, n):
        self.act(st[:, 1:2], st[:, 0:1], AF.Sqrt, [st], [st], scale=1.0 / n, bias=self.epsb[:, 0:1])
        self.recip(st[:, 1:2], st[:, 1:2], [st], [st])


class _Done(Exception):
    pass


def build_program(stop=None, dumps=()):
    nc = bass.Bass("TRN2", target_bir_lowering=False)
    small_peer = stop is not None

    def din(name, shape, dt=F32):
        return nc.dram_tensor(name, list(shape), dt, kind="ExternalInput").ap()

    xs = din("xs", [4096, 1024])
    pos_d = din("pos", [128, 32], I32)
    invf_d = din("invf", [128, 32])
    c_d = din("c_l", [128, 8])
    wada_d = din("w_ada", [1024, 6144])
    bada_d = din("b_ada", [128, 48])
    g1_d = din("g1", [128, 8])
    g2_d = din("g2", [128, 8])
    gq_d = din("gq", [128, 2])
    gkv_d = din("gkv", [128, 1])
    gout_d = din("gout", [128, 8])
    gsg_d = din("gsg", [1, 512])
    gfin_d = din("gfin", [1, 1024])
    bsg_d = din("bsgT", [128, 4])
    win_d = din("w_in", [1024, 1472])
    wuq_d = din("w_uq", [256, 768])
    wukv_d = din("w_ukv", [128, 1024])
    wsg_d = din("w_sgT", [128, 4, 128])
    wo_d = din("w_o", [1024, 1024])
    wpq_d = din("w_pq", [1024, 2048])
    keys_d = din("keysT", [128, 16, 128])
    put_d = din("peer_uT", [1024, 128 if small_peer else 16384])
    pv_d = din("peer_v", [128 if small_peer else 16384, 1024])
    out_d = nc.dram_tensor("out", [2048, 1024], F32, kind="ExternalOutput").ap()
    scr_d = nc.dram_tensor("scr", [56, 128], F32, kind="Internal").ap()
    scr_flat = scr_d.rearrange("a b -> (a b)")

    try:
      with ExitStack() as es0:
        P = Prog(nc, es0)
        k = K(P)
        scrB = Buf("scr", scr_d)
        reg = {}

        def finish(phase):
            if stop != phase:
                return
            tags = []
            for i, nm in enumerate(dumps):
                b = reg[nm]
                ap = b[:]
                od = nc.dram_tensor("dbg_" + nm, list(ap.shape), ap.dtype, kind="ExternalOutput").ap()
                tags.append(P.dma("sp", (lambda o, a: lambda e: e.dma_start(out=o, in_=a))(od, ap), "dbg%d" % i, [b], []))
            P.final_wait("sp", tags)
            print("total ops at finish", P.total, {e: P.cnt[e] for e in ENGS})
            import os
            if os.environ.get('SHOWLOG'):
                a, b = [int(v) for v in os.environ['SHOWLOG'].split(',')]
                for it in P.log:
                    if a <= it[0] <= b:
                        print(it)
            P.emit()
            raise _Done()

        identb = P.sb("identb", [128, 128], BF16)
        identf = P.sb("identf", [128, 128], F32)
        iotf = P.sb("iotf", [128, 128], F32)
        iot16 = P.sb("iot16", [128, 16], F32)
        fm = P.sb("fm", [128, 56], F32)
        g1s = P.sb("g1s", [128, 8], F32)
        smallw = P.sb("smallw", [128, 32], F32)
        bsgT = P.sb("bsgT_sb", [128, 4], F32)
        sts = [P.sb("st%d" % i, [128, 2], F32) for i in range(6)]
        epsb = P.sb("epsb", [128, 1], F32)
        k.memset(epsb[:], EPS, [epsb])
        k.epsb = epsb
        Y = P.sb("Y", [128, 16, 1024], BF16)

        P.op("pool", lambda e: e.iota(iotf[:], pattern=[[1, 128]], base=0, channel_multiplier=-1, allow_small_or_imprecise_dtypes=True), [], [iotf])
        P.op("pool", lambda e: e.iota(iot16[:], pattern=[[1, 16]], base=0, channel_multiplier=0, allow_small_or_imprecise_dtypes=True), [], [iot16])
        P.op("dve", lambda e: e.tensor_single_scalar(out=identf[:], in_=iotf[:], scalar=0.0, op=ALU.is_equal), [iotf], [identf])
        k.cp(identb[:], identf[:], [identf], [identb])
        k.dma("sp", smallw[:, 0:8], g1_d, "smallw", [], [smallw])
        k.dma("sp", smallw[:, 8:16], g2_d, "smallw", [], [smallw])
        k.dma("sp", smallw[:, 16:18], gq_d, "smallw", [], [smallw])
        k.dma("sp", smallw[:, 18:19], gkv_d, "smallw", [], [smallw])
        k.dma("sp", smallw[:, 19:27], gout_d, "smallw", [], [smallw])
        k.dma("sp", bsgT[:], bsg_d, "bsg", [], [bsgT])

        with ExitStack() as es:
            P.es = es
            cact = P.sb("cact", [128, 8, 2], F32)
            cl = P.sb("cl", [128, 8], F32)
            bada = P.sb("bada", [128, 48], F32)
            wa = [P.sb("wa%d" % i, [128, 8, 512], F32) for i in range(2)]
            modp = P.ps("modp", [128, 48, 2], F32)
            fmT_p = P.ps("fmT_p", [56, 128], F32)
            fmT = P.sb("fmT", [56, 128], F32)
            k.dma("sp", cl[:], c_d, "cl", [], [cl])
            k.dma("sp", bada[:], bada_d, "bada", [], [bada])
            k.act(cact[:, :, 0], cl[:], AF.Silu, [cl], [cact])
            k.act(cact[:, :, 1], cl[:], AF.Silu, [cl], [cact])
            wada_v = wada_d.rearrange("(k p) c -> p k c", p=128)
            for j in range(12):
                w = wa[j % 2]
                k.dma("sp", w[:], wada_v[:, :, j * 512:(j + 1) * 512], "wa%d" % (j % 2), [], [w])
                for jj in range(4):
                    col = j * 4 + jj
                    for kc in range(8):
                        k.mm(modp[:, col, :], w[:, kc, jj * 128:(jj + 1) * 128], cact[:, kc, :], kc == 0, kc == 7, [w, cact], [modp])
            k.tt(fm[:, 0:48], modp[:, :, 0], bada[:], ALU.add, [modp, bada], [fm])
            k.stt(g1s[:], fm[:, 8:16], 1.0, smallw[:, 0:8], ALU.add, ALU.mult, [fm, smallw], [g1s])
            k.stt(fm[:, 48:56], fm[:, 32:40], 1.0, smallw[:, 8:16], ALU.add, ALU.mult, [fm, smallw], [fm])
            k.tr(fmT_p[:], fm[:, 0:56], identf[:], [fm, identf], [fmT_p])
            k.cp(fmT[:], fmT_p[:], [fmT_p], [fmT])
            k.dma("sp", scr_d, fmT[:], "scrw", [fmT], [scrB])
            P.barrier()
            reg.update(fm=fm, g1s=g1s)
            finish("0")
        P.es = es0

        def bc_row(name, lo, n):
            t = P.sb(name, [128, n], F32)
            k.dma("sp", t[:], scr_flat[lo:lo + n].partition_broadcast(128), name, [scrB], [t])
            return t

        with ExitStack() as es12:
            P.es = es12
            KTn = P.sb("KTn", [128, 4, 4096], BF16)
            Vsb = P.sb("Vsb", [128, 32, 4, 129], BF16)
            KTr = P.sb("KTr", [128, 4096], BF16)
            cqT = P.sb("cqT", [128, 2, 2048], BF16)
            cosT = P.sb("cosT", [128, 32, 32], F32)
            sinT = P.sb("sinT", [128, 32, 32], F32)
            ssqa = P.sb("ssqa", [128, 16, 2], F32)
            k.memset(Vsb[:, :, :, 128:129], 1.0, [Vsb])
            k.memset(ssqa[:], 0.0, [ssqa])

            with ExitStack() as es:
                P.es = es
                Win = P.sb("Win", [128, 8, 1472], BF16)
                Wukv = P.sb("Wukv", [128, 1024], BF16)
                WsgT = P.sb("WsgT", [128, 4, 128], BF16)
                gsgbc = P.sb("gsgbc", [128, 512], F32)
                xt = [P.sb("xt%d" % i, [128, 1024], F32) for i in range(2)]
                xn = P.sb("xn", [128, 1024], BF16)
                hT = P.sb("hT", [128, 8, 128], BF16)
                z = P.sb("z", [128, 1472], F32)
                junk = P.sb("junk", [128, 512], BF16)
                ckvn = P.sb("ckvn", [128, 128], BF16)
                ckvT = P.sb("ckvT", [128, 128], BF16)
                kr = P.sb("kr", [128, 128], BF16)
                rt = [P.sb("rt%d" % i, [128, 32], F32) for i in range(4)]
                cqn = P.sb("cqn", [128, 256], BF16)
                gu = P.sb("gu", [128, 512], F32)
                gv = P.sb("gv", [128, 512], F32)
                vn = P.sb("vn", [128, 512], BF16)
                ysg = P.sb("ysg", [128, 512], F32)
                tp = P.ps("tp", [128, 8, 128], BF16)
                zp = P.ps("zp", [128, 1536], F32)
                tpx = P.ps("tpx", [128, 4, 128], BF16)
                kp = P.ps("kp", [128, 4, 128], F32)
                vp = P.ps("vp", [128, 512], F32)
                mp = P.ps("mp", [128, 4, 128], F32)
                st_x, st_kv, st_q, st_v, st_sg = sts[0], sts[1], sts[2], sts[3], sts[4]

                k.memset(kr[:], 0.0, [kr])
                k.dma("pool", Win[:], win_d.rearrange("(k p) c -> p k c", p=128), "Win", [], [Win])
                k.dma("pool", Wukv[:], wukv_d, "Wukv", [], [Wukv])
                k.dma("pool", WsgT[:], wsg_d, "WsgT", [], [WsgT])
                k.dma("sp", gsgbc[:], gsg_d.partition_broadcast(128), "gsgbc", [], [gsgbc])
                posi = P.sb("posi", [128, 32], I32)
                posf = P.sb("posf", [128, 32], F32)
                invf = P.sb("invf_sb", [128, 32], F32)
                ang = P.sb("ang", [128, 32, 32], F32)
                angi = P.sb("angi", [128, 32, 32], I32)
                angr = P.sb("angr", [128, 32, 32], F32)
                k.dma("sp", posi[:], pos_d, "posi", [], [posi])
                k.dma("sp", invf[:], invf_d, "invf", [], [invf])
                k.cp(posf[:], posi[:], [posi], [posf])
                k.tt(ang[:], posf[:].unsqueeze(2).to_broadcast([128, 32, 32]), invf[:].unsqueeze(1).to_broadcast([128, 32, 32]), ALU.mult, [posf, invf], [ang])
                for (dst, off) in ((sinT, 0.0), (cosT, 0.25)):
                    if off != 0.0:
                        k.ts(ang[:], ang[:], off, None, ALU.add, None, [ang], [ang])
                    k.cp(angi[:], ang[:], [ang], [angi])
                    k.cp(angr[:], angi[:], [angi], [angr])
                    k.tt(angr[:], ang[:], angr[:], ALU.subtract, [ang, angr], [angr])
                    k.act(dst[:], angr[:], AF.Sin, [angr], [dst], scale=6.28318)
                k.ts(Wukv[:], Wukv[:], smallw[:, 18:19], None, ALU.mult, None, [Wukv, smallw], [Wukv])

                for n in range(NT_SEQ):
                    own = n < NT_OWN
                    x_ = xt[n % 2]
                    k.dma("sp", x_[:], xs[n * 128:(n + 1) * 128, :], "xt%d" % (n % 2), [], [x_])
                    k.act(xn[:], x_[:], AF.Square, [x_], [xn, st_x], accum_out=st_x[:, 0:1])
                    k.rstd(st_x, 1024)
                    k.act(xn[:], x_[:], AF.Identity, [x_, st_x], [xn], scale=st_x[:, 1:2])
                    for kc in range(8):
                        k.tr(tp[:, kc, :], xn[:, kc * 128:(kc + 1) * 128], identb[:], [xn, identb], [tp])
                    for kc in range(8):
                        k.ts(hT[:, kc, :], tp[:, kc, :], g1s[:, kc:kc + 1], fm[:, kc:kc + 1], ALU.mult, ALU.add, [tp, g1s, fm], [hT])
                    chunks = ((0, 512), (512, 1024), (1024, 1472)) if own else ((256, 448),)
                    for (lo, hi) in chunks:
                        for kc in range(8):
                            k.mm(zp[:, lo:hi], hT[:, kc, :], Win[:, kc, lo:hi], kc == 0, kc == 7, [hT, Win], [zp])
                    for (lo, hi) in chunks:
                        k.act(z[:, lo:hi], zp[:, lo:hi], AF.Identity, [zp], [z])
                    k.act(junk[:, 0:128], z[:, 256:384], AF.Square, [z], [junk, st_kv], accum_out=st_kv[:, 0:1])
                    k.rstd(st_kv, 128)
                    k.act(ckvn[:], z[:, 256:384], AF.Identity, [z, st_kv], [ckvn], scale=st_kv[:, 1:2])
                    cs, sn = cosT[:, n, :], sinT[:, n, :]
                    x1, x2 = z[:, 384:416], z[:, 416:448]
                    k.tt(rt[0][:], x1, cs, ALU.mult, [z, cosT], [rt[0]])
                    k.tt(rt[1][:], x2, sn, ALU.mult, [z, sinT], [rt[1]])
                    k.tt(kr[:, 0:32], rt[0][:], rt[1][:], ALU.subtract, [rt[0], rt[1]], [kr])
                    k.tt(rt[2][:], x2, cs, ALU.mult, [z, cosT], [rt[2]])
                    k.tt(rt[3][:], x1, sn, ALU.mult, [z, sinT], [rt[3]])
                    k.tt(kr[:, 32:64], rt[2][:], rt[3][:], ALU.add, [rt[2], rt[3]], [kr])
                    k.tr(tpx[:, 0, :], ckvn[:], identb[:], [ckvn, identb], [tpx])
                    k.tr(tpx[:, 1, :], kr[:], identb[:], [kr, identb], [tpx])
                    k.cp(ckvT[:], tpx[:, 0, :], [tpx], [ckvT])
                    k.cp(KTr[:, n * 128:(n + 1) * 128], tpx[:, 1, :], [tpx], [KTr])
                    for h in range(4):
                        k.mm(kp[:, h, :], Wukv[:, h * 128:(h + 1) * 128], ckvT[:], True, True, [Wukv, ckvT], [kp])
                    k.mm(vp[:], ckvT[:], Wukv[:, 512:1024], True, True, [Wukv, ckvT], [vp])
                    k.act(KTn[:, :, n * 128:(n + 1) * 128], kp[:], AF.Identity, [kp], [KTn])
                    k.cp(Vsb[:, n, :, 0:128], vp[:].rearrange("p (h d) -> p h d", h=4), [vp], [Vsb])
                    if not own:
                        continue
                    k.act(junk[:, 0:256], z[:, 0:256], AF.Square, [z], [junk, st_q], accum_out=st_q[:, 0:1])
                    k.rstd(st_q, 256)
                    k.act(cqn[:], z[:, 0:256], AF.Identity, [z, st_q], [cqn], scale=st_q[:, 1:2])
                    for j in range(2):
                        k.tr(tpx[:, 2 + j, :], cqn[:, j * 128:(j + 1) * 128], identb[:], [cqn, identb], [tpx])
                    k.cp(cqT[:, :, n * 128:(n + 1) * 128], tpx[:, 2:4, :], [tpx], [cqT])
                    k.act(gu[:], z[:, 448:960], AF.Gelu, [z], [gu])
                    k.act(gv[:], z[:, 960:1472], AF.Gelu, [z], [gv])
                    k.act(junk[:], gv[:], AF.Square, [gv], [junk, st_v], accum_out=st_v[:, 0:1])
                    k.rstd(st_v, 512)
                    k.stt(vn[:], gv[:], st_v[:, 1:2], gsgbc[:], ALU.mult, ALU.mult, [gv, st_v, gsgbc], [vn])
                    for h in range(4):
                        k.mm(mp[:, h, :], WsgT[:, h, :], vn[:, h * 128:(h + 1) * 128], True, True, [WsgT, vn], [mp])
                    for h in range(4):
                        k.stt(ysg[:, h * 128:(h + 1) * 128], mp[:, h, :], bsgT[:, h:h + 1], gu[:, h * 128:(h + 1) * 128], ALU.add, ALU.mult, [mp, bsgT, gu], [ysg])
                    k.act(junk[:], ysg[:], AF.Square, [ysg], [junk, st_sg], accum_out=st_sg[:, 0:1])
                    k.rstd(st_sg, 512)
                    k.act(Y[:, n, 512:1024], ysg[:], AF.Identity, [ysg, st_sg], [Y], scale=st_sg[:, 1:2])
                P.barrier()
                reg.update(KTn=KTn, Vsb=Vsb, KTr=KTr, cqT=cqT, Y=Y)
                finish("1a")
            P.es = es12

            with ExitStack() as es:
                P.es = es
                Wuq = P.sb("Wuq", [128, 2, 768], BF16)
                QTn2 = [P.sb("QTn%d" % i, [128, 4, 512], BF16) for i in range(2)]
                QTr2 = [P.sb("QTr%d" % i, [128, 4, 512], BF16) for i in range(2)]
                qbr = P.sb("qbr", [128, 4, 128], BF16)
                qsb = P.sb("qsb", [128, 4, 192], F32)
                qb = P.sb("qb", [128, 4, 192], BF16)
                qt_ = [P.sb("qt%d" % i, [128, 4, 32], F32) for i in range(4)]
                PT = [P.sb("PT%d" % i, [128, 512], BF16) for i in range(4)]
                ya = P.sb("ya", [128, 128], F32)
                osb = P.sb("osb", [128, 4, 129], F32)
                rc = P.sb("rc", [128, 2], F32)
                jk2 = P.sb("jk2", [128, 128], BF16)
                qp = P.ps("qp", [128, 1024], F32)
                tq = P.ps("tq", [128, 8, 128], BF16)
                Sp = [P.ps("Sp%d" % i, [128, 512], F32) for i in range(3)]
                Op = P.ps("Op", [128, 4, 256], F32)
                st_a = sts[0]
                k.memset(qbr[:], 0.0, [qbr])
                k.dma("pool", Wuq[:], wuq_d.rearrange("(j p) c -> p j c", p=128), "Wuq", [], [Wuq])
                for j in range(2):
                    k.ts(Wuq[:, j, :], Wuq[:, j, :], smallw[:, 16 + j:17 + j], None, ALU.mult, None, [Wuq, smallw], [Wuq])
                sc = 192.0 ** -0.5
                si = 0
                def Qcomp(T):
                    QTn, QTr = QTn2[T % 2], QTr2[T % 2]
                    for jj in range(4):
                        n = T * 4 + jj
                        for (lo, hi) in ((0, 512), (512, 768)):
                            for j in range(2):
                                k.mm(qp[:, lo:hi], cqT[:, j, n * 128:(n + 1) * 128], Wuq[:, j, lo:hi], j == 0, j == 1, [cqT, Wuq], [qp])
                        k.act(qsb[:].rearrange("p h d -> p (h d)"), qp[:, 0:768], AF.Identity, [qp], [qsb], scale=sc)
                        cs = cosT[:, n, :].unsqueeze(1).to_broadcast([128, 4, 32])
                        sn = sinT[:, n, :].unsqueeze(1).to_broadcast([128, 4, 32])
                        x1, x2 = qsb[:, :, 128:160], qsb[:, :, 160:192]
                        k.tt(qt_[0][:], x1, cs, ALU.mult, [qsb, cosT], [qt_[0]])
                        k.tt(qt_[1][:], x2, sn, ALU.mult, [qsb, sinT], [qt_[1]])
                        k.tt(qbr[:, :, 0:32], qt_[0][:], qt_[1][:], ALU.subtract, [qt_[0], qt_[1]], [qbr])
                        k.tt(qt_[2][:], x2, cs, ALU.mult, [qsb, cosT], [qt_[2]])
                        k.tt(qt_[3][:], x1, sn, ALU.mult, [qsb, sinT], [qt_[3]])
                        k.tt(qbr[:, :, 32:64], qt_[2][:], qt_[3][:], ALU.add, [qt_[2], qt_[3]], [qbr])
                        k.act(qb[:, :, 0:128], qsb[:, :, 0:128], AF.Identity, [qsb], [qb])
                        for h in range(4):
                            k.tr(tq[:, h, :], qb[:, h, 0:128], identb[:], [qb, identb], [tq])
                            k.tr(tq[:, 4 + h, :], qbr[:, h, :], identb[:], [qbr, identb], [tq])
                        k.cp(QTn[:, :, jj * 128:(jj + 1) * 128], tq[:, 0:4, :], [tq], [QTn])
                        k.cp(QTr[:, :, jj * 128:(jj + 1) * 128], tq[:, 4:8, :], [tq], [QTr])

                Qcomp(0)
                for T in range(4):
                    QTn, QTr = QTn2[T % 2], QTr2[T % 2]
                    items = [(h, tk) for h in range(4) for tk in range(32)]

                    def emitS(q):
                        h, tk = items[q]
                        S = Sp[q % 3]
                        pt = PT[q % 4]
                        k.mm(S[:], KTn[:, h, tk * 128:(tk + 1) * 128], QTn[:, h, :], True, False, [KTn, QTn], [S])
                        k.mm(S[:], KTr[:, tk * 128:(tk + 1) * 128], QTr[:, h, :], False, True, [KTr, QTr], [S])
                        k.act(pt[:], S[:], AF.Exp, [S], [pt])

                    def emitPV(q):
                        h, tk = items[q]
                        pt = PT[q % 4]
                        for jj in range(4):
                            k.mm(Op[:, jj, 0:129], pt[:, jj * 128:(jj + 1) * 128], Vsb[:, tk, h, :], (tk == 0 and jj in (0, 2)), tk == 31, [pt, Vsb], [Op])
                        if tk == 31:
                            k.cp(osb[:], Op[:, :, 0:129], [Op], [osb])
                            for jj in range(4):
                                n = T * 4 + jj
                                k.recip(rc[:, 0:1], osb[:, jj, 128:129], [osb], [rc])
                                k.ts(ya[:], osb[:, jj, 0:128], rc[:, 0:1], None, ALU.mult, None, [osb, rc], [ya])
                                k.act(jk2[:], ya[:], AF.Square, [ya], [jk2, rc], accum_out=rc[:, 1:2])
                                k.tt(ssqa[:, n, 0:1], ssqa[:, n, 0:1], rc[:, 1:2], ALU.add, [ssqa, rc], [ssqa])
                                k.cp(Y[:, n, h * 128:(h + 1) * 128], ya[:], [ya], [Y])

                    emitS(0)
                    emitS(1)
                    for q in range(len(items)):
                        if q + 2 < len(items):
                            emitS(q + 2)
                        emitPV(q)
                        if q == 40 and T + 1 < 4:
                            Qcomp(T + 1)
                for n in range(NT_OWN):
                    k.act(st_a[:, 1:2], ssqa[:, n, 0:1], AF.Sqrt, [ssqa, epsb], [st_a], scale=1.0 / 512, bias=epsb[:, 0:1])
                    k.recip(st_a[:, 1:2], st_a[:, 1:2], [st_a], [st_a])
                    k.act(Y[:, n, 0:512], Y[:, n, 0:512], AF.Identity, [Y, st_a], [Y], scale=st_a[:, 1:2])
                P.barrier()
                finish("2")
            P.es = es12
        P.es = es0

        x1 = P.sb("x1", [128, 16, 1024], F32)
        x1b = [Buf("x1_%d" % n, x1.t[:, n, :]) for n in range(NT_OWN)]
        rstd2 = P.sb("rstd2", [128, 16], F32)
        with ExitStack() as es:
            P.es = es
            Wo = P.sb("Wo", [128, 8, 1024], BF16)
            g1bc = bc_row("g1bc", 16 * 128, 1024)
            xt = [P.sb("xt3_%d" % i, [128, 1024], F32) for i in range(2)]
            YT2 = [P.sb("YT%d" % i, [128, 8, 128], BF16) for i in range(2)]
            tmp = P.sb("tmp3", [128, 1024], F32)
            tp2 = [P.ps("tp3_%d" % i, [128, 8, 128], BF16) for i in range(2)]
            op2 = [P.ps("op3_%d" % i, [128, 1024], F32) for i in range(2)]
            k.dma("pool", Wo[:], wo_d.rearrange("(k p) c -> p k c", p=128), "Wo", [], [Wo])
            for kc in range(8):
                k.ts(Wo[:, kc, :], Wo[:, kc, :], smallw[:, 19 + kc:20 + kc], None, ALU.mult, None, [Wo, smallw], [Wo])

            def p3A(n):
                x_ = xt[n % 2]
                k.dma("sp", x_[:], xs[n * 128:(n + 1) * 128, :], "xt3_%d" % (n % 2), [], [x_])
                for kc in range(8):
                    k.tr(tp2[n % 2][:, kc, :], Y[:, n, kc * 128:(kc + 1) * 128], identb[:], [Y, identb], [tp2[n % 2]])
                k.cp(YT2[n % 2][:], tp2[n % 2][:], [tp2[n % 2]], [YT2[n % 2]])

            def p3B(n):
                x_ = xt[n % 2]
                YT, op_ = YT2[n % 2], op2[n % 2]
                for c in range(2):
                    for kc in range(8):
                        k.mm(op_[:, c * 512:(c + 1) * 512], YT[:, kc, :], Wo[:, kc, c * 512:(c + 1) * 512], kc == 0, kc == 7, [YT, Wo], [op_])
                k.tt(tmp[:], op_[:], g1bc[:], ALU.mult, [op_, g1bc], [tmp])
                k.tt(x1b[n][:], tmp[:], x_[:], ALU.add, [tmp, x_], [x1b[n]])

            p3A(0)
            for n in range(NT_OWN):
                if n + 1 < NT_OWN:
                    p3A(n + 1)
                p3B(n)
            P.barrier()
            reg.update(x1=x1)
            finish("3")
        P.es = es0

        with ExitStack() as es4:
            P.es = es4
            H2T = P.sb("H2T", [128, 8, 2048], BF16)
            es4ab = ExitStack()
            P.es = es4ab
            tabT = P.sb("tabT", [128, 16, 3, 128], BF16)
            P.es = es4
            with ExitStack() as es:
                P.es = es
                Wpq = Buf("Wpq", Y.t[:].rearrange("p a (b c) -> p (a b) c", b=2).rearrange("p (k j) c -> p k (j c)", k=8))
                keysT = P.sb("keysT", [128, 16, 128], F32)
                xn2 = P.sb("xn2", [128, 1024], BF16)
                qTs2 = [P.sb("qTs%d" % i, [128, 16, 128], F32) for i in range(1)] * 2
                ssb2 = [P.sb("ssb%d" % i, [128, 16, 128], F32) for i in range(2)]
                qTs = qTs2[0]
                v16 = P.sb("v16", [128, 8, 2, 16], F32)
                ix = P.sb("ix", [128, 8, 2, 16], U32)
                ixf = P.sb("ixf", [128, 8, 2, 16], F32)
                cand = P.sb("cand", [128, 8, 16, 16], F32)
                scr8 = P.sb("scr8", [128, 8, 256], F32)
                cv = P.sb("cvv", [128, 8, 16], F32)
                pos = P.sb("pos4", [128, 8, 16], U32)
                pa = P.sb("pa", [128, 8, 16], U32)
                pb = P.sb("pb", [128, 8, 16], U32)
                paf = P.sb("paf", [128, 8, 16], F32)
                pbf = P.sb("pbf", [128, 8, 16], F32)
                i1s = P.sb("i1s", [128, 8, 16], F32)
                i2s = P.sb("i2s", [128, 8, 16], F32)
                ex = P.sb("ex", [128, 8, 16], F32)
                zz = P.sb("zz", [128, 8], F32)
                tabm = P.sb("tabm", [128, 3, 128], BF16)
                tp = P.ps("tp4", [128, 8, 128], BF16)
                qp = P.ps("qp4", [128, 16, 128], F32)
                tbp = P.ps("tbp", [128, 3, 128], BF16)
                st2 = sts[0]
                candw = scr8
                v16c = [Buf("v16c%d" % c, v16.t[:, c // 2, c % 2, :]) for c in range(16)]
                ixc = [Buf("ixc%d" % c, ix.t[:, c // 2, c % 2, :]) for c in range(16)]
                swc2 = [[Buf("swc%d_%d" % (i, c), ssb2[i].t[:, c, :]) for c in range(16)] for i in range(2)]
                cvh = [Buf("cvh%d" % h, cv.t[:, h, :]) for h in range(8)]
                posh = [Buf("posh%d" % h, pos.t[:, h, :]) for h in range(8)]
                cwh = [Buf("cwh%d" % h, scr8.t[:, h, :]) for h in range(8)]
                eq4 = Buf("eq4", scr8.t[:].rearrange("p h (a b) -> p h a b", a=16))
                eq4 = scr8
                eqv = scr8.t[:].rearrange("p h (a b) -> p h a b", a=16)
                k.dma("pool", Wpq[:], wpq_d.rearrange("(k p) c -> p k c", p=128), "Wpq", [], [Wpq])
                k.dma("sp", keysT[:], keys_d, "keysT", [], [keysT])
                def front4a(n):
                    xb = x1b[n]
                    qTs, ssb, swc = qTs2[n % 2], ssb2[n % 2], swc2[n % 2]
                    sw = ssb
                    k.act(xn2[:], xb[:], AF.Square, [xb], [xn2, st2], accum_out=st2[:, 0:1])
                    k.rstd(st2, 1024)
                    k.act(xn2[:], xb[:], AF.Identity, [xb, st2], [xn2], scale=st2[:, 1:2])
                    for kc in range(8):
                        k.tr(tp[:, kc, :], xn2[:, kc * 128:(kc + 1) * 128], identb[:], [xn2, identb], [tp])
                    for kc in range(8):
                        k.act(H2T[:, kc, n * 128:(n + 1) * 128], tp[:, kc, :], AF.Identity, [tp, fm], [H2T], scale=fm[:, 48 + kc:49 + kc], bias=fm[:, 24 + kc:25 + kc])
                    for c in range(16):
                        for kc in range(8):
                            k.mm(qp[:, c, :], Wpq[:, kc, c * 128:(c + 1) * 128], H2T[:, kc, n * 128:(n + 1) * 128], kc == 0, kc == 7, [Wpq, H2T], [qp])
                    k.act(qTs[:], qp[:], AF.Identity, [qp], [qTs])
                    for c in range(16):
                        k.mm(qp[:, c, :], qTs[:, c, :], keysT[:, c, :], True, True, [qTs, keysT], [qp])
                    k.act(ssb[:], qp[:], AF.Identity, [qp], [ssb] + swc)

                def back4a(n):
                    qTs, ssb, swc = qTs2[n % 2], ssb2[n % 2], swc2[n % 2]
                    sw = ssb
                    hs = [(c // 2, c % 2) for c in range(16)]
                    for c, (h, s_) in enumerate(hs):
                        P.op("dve", (lambda o, i: lambda e: e.max(out=o, in_=i))(v16[:, h, s_, 0:8], ssb[:, c, :]), [ssb, swc[c]], [v16c[c]])
                    for c, (h, s_) in enumerate(hs):
                        P.op("dve", (lambda o, m, i: lambda e: e.max_index(out=o, in_max=m, in_values=i))(ix[:, h, s_, 0:8], v16[:, h, s_, 0:8], ssb[:, c, :]), [ssb, swc[c], v16c[c]], [ixc[c]])
                    for c, (h, s_) in enumerate(hs):
                        P.op("dve", (lambda o, m, i: lambda e: e.match_replace(out=o, in_to_replace=m, in_values=i, imm_value=-1e30))(sw[:, c, :], v16[:, h, s_, 0:8], ssb[:, c, :]), [ssb, swc[c], v16c[c]], [swc[c]])
                    for c, (h, s_) in enumerate(hs):
                        P.op("dve", (lambda o, i: lambda e: e.max(out=o, in_=i))(v16[:, h, s_, 8:16], sw[:, c, :]), [swc[c]], [v16c[c]])
                    for c, (h, s_) in enumerate(hs):
                        P.op("dve", (lambda o, m, i: lambda e: e.max_index(out=o, in_max=m, in_values=i))(ix[:, h, s_, 8:16], v16[:, h, s_, 8:16], sw[:, c, :]), [swc[c], v16c[c]], [ixc[c]])
                    k.cp(ixf[:], ix[:], ixc, [ixf])
                    k.tt(cand[:], v16[:, :, 0, :].unsqueeze(3).to_broadcast([128, 8, 16, 16]), v16[:, :, 1, :].unsqueeze(2).to_broadcast([128, 8, 16, 16]), ALU.add, v16c, [cand])
                    cfs = [cand[:, h].rearrange("p a b -> p (a b)") for h in range(8)]
                    for h in range(8):
                        P.op("dve", (lambda o, i: lambda e: e.max(out=o, in_=i))(cv[:, h, 0:8], cfs[h]), [cand], [cvh[h]])
                    for h in range(8):
                        P.op("dve", (lambda o, m, i: lambda e: e.max_index(out=o, in_max=m, in_values=i))(pos[:, h, 0:8], cv[:, h, 0:8], cfs[h]), [cand, cvh[h]], [posh[h]])
                    for h in range(8):
                        P.op("dve", (lambda o, m, i: lambda e: e.match_replace(out=o, in_to_replace=m, in_values=i, imm_value=-1e30))(candw[:, h, :], cv[:, h, 0:8], cfs[h]), [cand, cvh[h]], [cwh[h]])
                    for h in range(8):
                        P.op("dve", (lambda o, i: lambda e: e.max(out=o, in_=i))(cv[:, h, 8:16], candw[:, h, :]), [cwh[h]], [cvh[h]])
                    for h in range(8):
                        P.op("dve", (lambda o, m, i: lambda e: e.max_index(out=o, in_max=m, in_values=i))(pos[:, h, 8:16], cv[:, h, 8:16], candw[:, h, :]), [cwh[h], cvh[h]], [posh[h]])
                    P.op("dve", lambda e: e.tensor_single_scalar(out=pa[:], in_=pos[:], scalar=4, op=ALU.logical_shift_right), posh, [pa])
                    P.op("dve", lambda e: e.tensor_single_scalar(out=pb[:], in_=pos[:], scalar=15, op=ALU.bitwise_and), posh, [pb])
                    k.cp(paf[:], pa[:], [pa], [paf])
                    k.cp(pbf[:], pb[:], [pb], [pbf])
                    iob = iot16[:].unsqueeze(1).unsqueeze(1).to_broadcast([128, 8, 16, 16])
                    for (pf, side, dst) in ((paf, 0, i1s), (pbf, 1, i2s)):
                        k.tt(eqv, pf[:].unsqueeze(3).to_broadcast([128, 8, 16, 16]), iob, ALU.is_equal, [pf, iot16], [scr8] + cwh)
                        k.tt(eqv, eqv, ixf[:, :, side, :].unsqueeze(2).to_broadcast([128, 8, 16, 16]), ALU.mult, [scr8, ixf], [scr8])
                        P.op("dve", (lambda o, i: lambda e: e.tensor_reduce(out=o, in_=i, axis=AX.X, op=ALU.add))(dst[:], eqv), [scr8], [dst])
                    k.cp(tabm[:, 0, :], i1s[:].rearrange("p h k -> p (h k)"), [i1s], [tabm])
                    k.cp(tabm[:, 1, :], i2s[:].rearrange("p h k -> p (h k)"), [i2s], [tabm])
                    k.tt(ex[:], cv[:], cv[:, :, 0:1].to_broadcast([128, 8, 16]), ALU.subtract, cvh, [ex])
                    k.act(ex[:], ex[:], AF.Exp, [ex], [ex])
                    P.op("dve", (lambda o, i: lambda e: e.tensor_reduce(out=o, in_=i, axis=AX.X, op=ALU.add))(zz[:], ex[:]), [ex], [zz])
                    k.recip(zz[:], zz[:], [zz], [zz])
                    k.tt(tabm[:, 2, :].rearrange("p (h k) -> p h k", h=8), ex[:], zz[:].unsqueeze(2).to_broadcast([128, 8, 16]), ALU.mult, [ex, zz], [tabm])
                    for q in range(3):
                        k.tr(tbp[:, q, :], tabm[:, q, :], identb[:], [tabm, identb], [tbp])
                    k.cp(tabT[:, n, :, :], tbp[:], [tbp], [tabT])

                front4a(0)
                for n in range(NT_OWN):
                    if n + 1 < NT_OWN:
                        front4a(n + 1)
                    back4a(n)
                P.barrier()
                reg.update(tabT=tabT, H2T=H2T)
                finish("4a")
            P.es = es4

            Gd = nc.dram_tensor("Gd", [16, 128, 128, 128], BF16, kind="Internal").ap()
            GdB = Buf("Gd", Gd)
            with ExitStack() as es:
                P.es = es
                Gt = Buf("Gt", Y.t[:].rearrange("p a (b c) -> p (a b) c", b=8))
                iotab = P.sb("iotab", [128, 128], BF16)
                TB = 16
                Lr = [P.sb("Lr%d" % i, [128, TB, 128], BF16) for i in range(2)]
                Rr = [P.sb("Rr%d" % i, [128, TB, 128], BF16) for i in range(2)]
                gps = [P.ps("gps%d" % i, [128, 8, 128], F32) for i in range(2)]
                Gt1 = P.sb("Gt1", [128, 128, 128], BF16)
                Gts = [Gt, Gt1]
                P.op("pool", lambda e: e.iota(iotab[:], pattern=[[1, 128]], base=0, channel_multiplier=0, allow_small_or_imprecise_dtypes=True), [], [iotab])
                ri = 0
                for n in range(NT_OWN):
                    Gtn = Gts[n % 2]
                    for t0 in range(0, 128, TB):
                        L, R = Lr[ri % 2], Rr[ri % 2]
                        ri += 1
                        for q in range(TB):
                            t = t0 + q
                            k.ts(L[:, q, :], iotab[:], tabT[:, n, 0, t:t + 1], tabT[:, n, 2, t:t + 1], ALU.is_equal, ALU.mult, [iotab, tabT], [L])
                            k.ts(R[:, q, :], iotab[:], tabT[:, n, 1, t:t + 1], None, ALU.is_equal, None, [iotab, tabT], [R])
                        for q in range(TB):
                            t = t0 + q
                            g = gps[(t // 8) % 2]
                            k.mm(g[:, t % 8, :], R[:, q, :], L[:, q, :], True, True, [R, L], [g])
                            if t % 8 == 7:
                                k.act(Gtn[:, :, t - 7:t + 1], g[:].rearrange("j t i -> j i t"), AF.Identity, [g], [Gtn])
                    k.dma("sp", Gd[n], Gtn[:], "Gdw%d" % (n % 2), [Gtn], [GdB])
                Gt = Gts[(NT_OWN - 1) % 2]
                P.barrier()
                reg.update(Gt=Gt)
                finish("4b")
            P.es = es4
            es4ab.close()

            with ExitStack() as es:
                P.es = es
                g2bc = bc_row("g2bc", 40 * 128, 1024)
                gfbc = P.sb("gfbc", [128, 1024], F32)
                k.dma("sp", gfbc[:], gfin_d.partition_broadcast(128), "gfbc", [], [gfbc])
                UTg = [Buf("UTg%d" % b, Y.t[:, 4 * b:4 * b + 4, :].rearrange("p a (b c) -> p (a b) c", b=2)) for b in range(2)]
                Vg = [Buf("Vg%d" % b, Y.t[:, 8 + 4 * b:12 + 4 * b, :]) for b in range(2)]
                xl = [P.sb("xl%d" % i, [128, 1024], F32) for i in range(2)]
                Gg = [P.sb("Gg%d" % i, [128, 16, 4, 128], BF16) for i in range(2)]
                ot = [P.sb("ot%d" % i, [128, 1024], F32) for i in range(2)]
                NAP = 3
                aps = [P.ps("aps%d" % i, [128, 512], F32) for i in range(NAP)]
                accs = [P.ps("acc%d" % i, [128, 1024], F32) for i in range(2)]
                st3 = sts[1]
                X1d = nc.dram_tensor("X1d", [16, 128, 1024], F32, kind="Internal").ap()
                X1dB = Buf("X1d", X1d)
                for n in range(NT_OWN):
                    k.dma("sp", X1d[n], x1b[n][:], "x1sp", [x1b[n]], [X1dB])
                UT_v = put_d.rearrange("(k p) e -> p k e", p=128)
                V_v = pv_d.rearrange("(i j) d -> j i d", j=128)
                Gd_v = Gd.rearrange("n j i t -> j n i t")
                NG = 32

                def load_group(g):
                    b = g % 2
                    k.dma("pool", UTg[b][:], UT_v[:, :, g * 512:(g + 1) * 512], "UTg%d" % b, [], [UTg[b]])
                    k.dma("pool", Vg[b][:], V_v[:, 4 * g:4 * g + 4, :], "Vg%d" % b, [], [Vg[b]])
                    k.dma("act", Gg[b][:], Gd_v[:, :, 4 * g:4 * g + 4, :], "Gg%d" % b, [GdB], [Gg[b]])

                steps = [(g, c) for g in range(NG) for c in range(8)]
                NGL, NWT = 4, 8
                gl = [P.sb("gl2_%d" % i, [128, 256], BF16) for i in range(NGL)]
                wt = [P.sb("wt2_%d" % i, [128, 256], BF16) for i in range(NWT)]

                def Ablock(si):
                    g, c = steps[si]
                    b = g % 2
                    for i in range(4):
                        slot = si * 4 + i
                        ap = aps[slot % NAP]
                        gg, ww = gl[slot % NGL], wt[slot % NWT]
                        for kc in range(8):
                            k.mm(ap[:, 0:256], UTg[b][:, kc, i * 128:(i + 1) * 128], H2T[:, kc, c * 256:(c + 1) * 256], kc == 0, kc == 7, [UTg[b], H2T], [ap])
                        k.act(gg[:], ap[:, 0:256], AF.Gelu, [ap], [gg])
                        k.tt(ww[:].rearrange("p (s t) -> p s t", s=2), gg[:].rearrange("p (s t) -> p s t", s=2), Gg[b][:, 2 * c:2 * c + 2, i, :], ALU.mult, [gg, Gg[b]], [ww])

                def Oblock(si):
                    g, c = steps[si]
                    b = g % 2
                    for s_ in range(2):
                        A = accs[s_]
                        for i in range(4):
                            ww = wt[(si * 4 + i) % NWT]
                            for dc in range(2):
                                k.mm(A[:, dc * 512:(dc + 1) * 512], ww[:, s_ * 128:(s_ + 1) * 128], Vg[b][:, i, dc * 512:(dc + 1) * 512], i == 0, i == 3, [ww, Vg[b]], [A])
                        xb = x1b[2 * c + s_]
                        if g == 0:
                            k.act(xb[:], A[:], AF.Identity, [A], [xb])
                        else:
                            k.tt(xb[:], A[:], xb[:], ALU.add, [A, xb], [xb])

                load_group(0)
                Ablock(0)
                for si in range(len(steps)):
                    g, c = steps[si]
                    if c == 0 and g + 1 < NG:
                        load_group(g + 1)
                    if si + 1 < len(steps):
                        Ablock(si + 1)
                    Oblock(si)
                outs = []
                st3s = [sts[1], sts[2]]

                def finA(n):
                    xb = x1b[n]
                    xo = xl[n % 2]
                    s3 = st3s[n % 2]
                    k.dma("sp", xo[:], X1d[n], "xl%d" % (n % 2), [X1dB], [xo])
                    k.tt(xb[:], xb[:], g2bc[:], ALU.mult, [xb, g2bc], [xb])
                    k.tt(xb[:], xb[:], xo[:], ALU.add, [xb, xo], [xb])
                    k.act(xo[:], xb[:], AF.Square, [xb], [xo, s3], accum_out=s3[:, 0:1])
                    k.act(s3[:, 1:2], s3[:, 0:1], AF.Sqrt, [s3], [s3], scale=1.0 / 1024, bias=epsb[:, 0:1])

                def finB(n):
                    xb = x1b[n]
                    s3 = st3s[n % 2]
                    k.recip(s3[:, 1:2], s3[:, 1:2], [s3], [s3])
                    o = ot[n % 2]
                    k.stt(o[:], xb[:], s3[:, 1:2], gfbc[:], ALU.mult, ALU.mult, [xb, s3, gfbc], [o])
                    outs.append(k.dma("sp", out_d[n * 128:(n + 1) * 128, :], o[:], "ot%d" % (n % 2), [o], []))

                finA(0)
                for n in range(NT_OWN):
                    if n + 1 < NT_OWN:
                        finA(n + 1)
                    finB(n)
                P.final_wait("sp", outs)
            P.es = es4
        P.es = es0
        P.emit()
    except _Done:
        pass
    return nc


def _prep_inputs(inp):
    f32 = np.float32
    x = np.asarray(inp["x"], f32)
    c = np.asarray(inp["c"], f32)
    positions = np.asarray(inp["positions"], np.int32)

    def fmaj(v, n):
        return np.ascontiguousarray(np.asarray(v, f32).reshape(n, 128).T)

    inv_freq = (1.0 / (10000.0 ** (np.arange(0, 64, 2, dtype=np.float32) / 64.0))).astype(f32)
    invf = np.ascontiguousarray(np.tile((inv_freq / np.float32(2 * np.pi)).astype(f32)[None, :], (128, 1)))
    w_ukv = np.asarray(inp["w_ukv"], f32)[0].reshape(128, 4, 2, 128)
    w_ukv_l = np.ascontiguousarray(np.concatenate([w_ukv[:, :, 0, :].reshape(128, 512), w_ukv[:, :, 1, :].reshape(128, 512)], axis=1))
    shared = {
        "invf": invf,
        "w_ada": np.ascontiguousarray(np.asarray(inp["w_ada"], f32)[0]),
        "b_ada": fmaj(np.asarray(inp["b_ada"])[0], 48),
        "g1": fmaj(np.asarray(inp["g_norm1"])[0], 8),
        "g2": fmaj(np.asarray(inp["g_norm2"])[0], 8),
        "gq": fmaj(np.asarray(inp["g_q_a"])[0], 2),
        "gkv": fmaj(np.asarray(inp["g_kv_a"])[0], 1),
        "gout": fmaj(np.concatenate([np.asarray(inp["g_attn_out"])[0], np.asarray(inp["g_sg_out"])[0]]), 8),
        "gsg": np.ascontiguousarray(np.asarray(inp["g_sg"], f32)[0].reshape(1, 512)),
        "gfin": np.ascontiguousarray(np.asarray(inp["g_final"], f32).reshape(1, 1024)),
        "bsgT": np.ascontiguousarray(np.asarray(inp["b_sg"], f32)[0].T),
        "w_in": np.ascontiguousarray(np.asarray(inp["w_in"], f32)[0]),
        "w_uq": np.ascontiguousarray(np.asarray(inp["w_uq"], f32)[0]),
        "w_ukv": w_ukv_l,
        "w_sgT": np.ascontiguousarray(np.asarray(inp["w_sg"], f32)[0].transpose(2, 0, 1)),
        "w_o": np.ascontiguousarray(np.asarray(inp["w_o"], f32)[0]),
        "w_pq": np.ascontiguousarray(np.asarray(inp["w_peer_q"], f32)[0]),
        "keysT": np.ascontiguousarray(np.asarray(inp["peer_keys"], f32)[0].reshape(16, 128, 128).transpose(2, 0, 1)),
        "peer_uT": np.ascontiguousarray(np.asarray(inp["peer_u"], f32)[0].T),
        "peer_v": np.ascontiguousarray(np.asarray(inp["peer_v"], f32)[0]),
    }
    maps = []
    for core in range(8):
        b, half = core // 2, core % 2
        own = slice(half * 2048, (half + 1) * 2048)
        oth = slice((1 - half) * 2048, (2 - half) * 2048)
        m = dict(shared)
        m["xs"] = np.ascontiguousarray(np.concatenate([x[b, own], x[b, oth]], axis=0))
        p = np.concatenate([positions[b, own], positions[b, oth]]).astype(np.int32)
        m["pos"] = np.ascontiguousarray(p.reshape(32, 128).T)
        m["c_l"] = fmaj(c[b], 8)
        maps.append(m)
    return maps


_NC = None


def kernel(**inputs):
    global _NC
    maps = _prep_inputs(inputs)
    if _NC is None:
        _NC = build_program()
    res = run_bass_kernel_spmd(_NC, maps, core_ids=list(range(8)))
    out = np.empty((4, 4096, 1024), np.float32)
    for core in range(8):
        b, half = core // 2, core % 2
        out[b, half * 2048:(half + 1) * 2048, :] = res.results[core]["out"]
    return out
```
